# Optimizing a Trainium2 kernel written in Bass

```python
import math
import jax, jax.numpy as jnp
from jax import lax
import numpy as np

D_MODEL = 1024
BATCH = 2
SEQ = 8192
DEPTH = 1

D_CONV = D_MODEL
CONV_WIDTH = 3
HEAD_DIM = 64
N_Q_HEADS = D_MODEL // HEAD_DIM
N_KV_HEADS = 4
GQA_GROUP = N_Q_HEADS // N_KV_HEADS
WINDOW = 128
BLOCK = 128
N_EXPERTS = 32
TOP_K = 4
D_EXPERT = D_MODEL
SWIGLU_LIMIT = 7.0
SWIGLU_ALPHA = 1.702
EXPERT_CHUNK = 256
D_PLE = 256
DN_ALPHA = (2.0 * DEPTH) ** 0.25
DN_BETA = (8.0 * DEPTH) ** -0.25
LN_EPS = 1e-5

IN_SIZES = [D_CONV, D_CONV, D_CONV,
            N_Q_HEADS * HEAD_DIM, N_KV_HEADS * HEAD_DIM, N_KV_HEADS * HEAD_DIM,
            D_MODEL, D_MODEL]
IN_TOTAL = sum(IN_SIZES)
IN_SPLITS = [int(s) for s in np.cumsum(IN_SIZES)[:-1]]
V_START = 3 * D_CONV + N_Q_HEADS * HEAD_DIM + N_KV_HEADS * HEAD_DIM
V_END = V_START + N_KV_HEADS * HEAD_DIM

kernel_name = "hybrid_conv_swa_moe_deepnorm"


def layer_norm(x, g, b):
    xf = x.astype(jnp.float32)
    mu = jnp.mean(xf, axis=-1, keepdims=True)
    var = jnp.mean(jnp.square(xf - mu), axis=-1, keepdims=True)
    y = (xf - mu) * lax.rsqrt(var + LN_EPS) * g.astype(jnp.float32) + b.astype(jnp.float32)
    return y.astype(x.dtype)


def short_conv_mixer(b_gate, c_gate, h, conv_w):
    u = c_gate * h
    S = u.shape[1]
    up = jnp.pad(u, ((0, 0), (CONV_WIDTH - 1, 0), (0, 0)))
    y = sum(conv_w[k] * up[:, k:k + S] for k in range(CONV_WIDTH))
    return b_gate * y


def sliding_window_attention(q, k, v, sinks):
    B, S, _ = q.shape
    nb = S // BLOCK
    qb = q.reshape(B, nb, BLOCK, N_KV_HEADS, GQA_GROUP, HEAD_DIM)
    k4 = k.reshape(B, S, N_KV_HEADS, HEAD_DIM)
    v4 = v.reshape(B, S, N_KV_HEADS, HEAD_DIM)
    pad = ((0, 0), (BLOCK, 0), (0, 0), (0, 0))
    kp = jnp.pad(k4, pad)[:, :S].reshape(B, nb, BLOCK, N_KV_HEADS, HEAD_DIM)
    vp = jnp.pad(v4, pad)[:, :S].reshape(B, nb, BLOCK, N_KV_HEADS, HEAD_DIM)
    kb = jnp.concatenate([kp, k4.reshape(B, nb, BLOCK, N_KV_HEADS, HEAD_DIM)], axis=2)
    vb = jnp.concatenate([vp, v4.reshape(B, nb, BLOCK, N_KV_HEADS, HEAD_DIM)], axis=2)
    scale = 1.0 / math.sqrt(HEAD_DIM)
    s = jnp.einsum('bnqhgd,bnkhd->bnhgqk', qb, kb).astype(jnp.float32) * scale
    qi = jnp.arange(BLOCK)[:, None] + BLOCK
    kj = jnp.arange(2 * BLOCK)[None, :]
    rel = qi - kj
    band = (rel >= 0) & (rel < WINDOW)
    valid_key = (jnp.arange(nb)[:, None] * BLOCK - BLOCK + kj) >= 0
    mask = band[None] & valid_key[:, None, :]
    s = jnp.where(mask[None, :, None, None], s, -jnp.inf)
    sink = sinks.astype(jnp.float32).reshape(1, 1, N_KV_HEADS, GQA_GROUP, 1, 1)
    m = jnp.maximum(jnp.max(s, axis=-1, keepdims=True), sink)
    e = jnp.exp(s - m)
    denom = jnp.sum(e, axis=-1, keepdims=True) + jnp.exp(sink - m)
    probs = (e / denom).astype(q.dtype)
    o = jnp.einsum('bnhgqk,bnkhd->bnqhgd', probs, vb)
    return o.reshape(B, S, N_Q_HEADS * HEAD_DIM)


def routed_experts(x, w_router, b_router, w_gu, b_gu, w_down, b_down):
    B, S, D = x.shape
    N = B * S
    t = x.reshape(N, D)
    logits = (t @ w_router + b_router).astype(jnp.float32)
    top_vals, top_idx = lax.top_k(logits, TOP_K)
    top_w = jax.nn.softmax(top_vals, axis=-1)
    A = N * TOP_K
    flat_e = top_idx.reshape(A)
    flat_tok = jnp.repeat(jnp.arange(N, dtype=jnp.int32), TOP_K)
    flat_w = top_w.reshape(A)
    order = jnp.argsort(flat_e, stable=True)
    sorted_e = flat_e[order]
    counts = jnp.bincount(flat_e, length=N_EXPERTS)
    padded = ((counts + EXPERT_CHUNK - 1) // EXPERT_CHUNK) * EXPERT_CHUNK
    start_sorted = jnp.cumsum(counts) - counts
    end_padded = jnp.cumsum(padded)
    start_padded = end_padded - padded
    rank = jnp.arange(A) - start_sorted[sorted_e]
    dest = start_padded[sorted_e] + rank
    P = A + N_EXPERTS * EXPERT_CHUNK
    P = ((P + EXPERT_CHUNK - 1) // EXPERT_CHUNK) * EXPERT_CHUNK
    n_chunks = P // EXPERT_CHUNK
    tok_buf = jnp.full((P,), N, dtype=jnp.int32).at[dest].set(flat_tok[order])
    w_buf = jnp.zeros((P,), jnp.float32).at[dest].set(flat_w[order])
    chunk_start = jnp.arange(n_chunks) * EXPERT_CHUNK
    chunk_e = jnp.clip(jnp.searchsorted(end_padded, chunk_start, side='right'), 0, N_EXPERTS - 1)
    t_pad = jnp.concatenate([t, jnp.zeros((1, D), t.dtype)], axis=0)
    xs = t_pad[tok_buf].reshape(n_chunks, EXPERT_CHUNK, D)

    def expert_chunk(args):
        xc, e = args
        gu = xc @ w_gu[e] + b_gu[e]
        gate, up = gu[:, :D_EXPERT], gu[:, D_EXPERT:]
        gate = jnp.minimum(gate, SWIGLU_LIMIT)
        up = jnp.clip(up, -SWIGLU_LIMIT, SWIGLU_LIMIT)
        h = (up + 1.0) * gate * jax.nn.sigmoid(SWIGLU_ALPHA * gate)
        return h @ w_down[e] + b_down[e]

    ys = lax.map(expert_chunk, (xs, chunk_e)).reshape(P, D)
    ys = ys * w_buf[:, None].astype(ys.dtype)
    out = jnp.zeros((N + 1, D), ys.dtype).at[tok_buf].add(ys)[:N]
    return out.reshape(B, S, D).astype(x.dtype)


def setup_inputs(seed: int = 0) -> dict:
    key = jax.random.key(seed)
    ks = jax.random.split(key, 24)
    f32 = jnp.float32
    nrm = lambda k, shape, s: jax.random.normal(k, shape, f32) * s
    L = DEPTH
    w_in = nrm(ks[2], (L, D_MODEL, IN_TOTAL), D_MODEL ** -0.5)
    w_in = w_in.at[:, :, V_START:V_END].multiply(DN_BETA)
    return {
        "x": nrm(ks[0], (BATCH, SEQ, D_MODEL), 1.0),
        "p": nrm(ks[1], (DEPTH, BATCH, SEQ, D_PLE), 1.0),
        "w_in": w_in,
        "conv_w": nrm(ks[3], (L, CONV_WIDTH, D_CONV), CONV_WIDTH ** -0.5),
        "w_br_conv": nrm(ks[4], (L, D_CONV, D_MODEL), D_CONV ** -0.5 * DN_BETA),
        "w_br_attn": nrm(ks[5], (L, N_Q_HEADS * HEAD_DIM, D_MODEL), (N_Q_HEADS * HEAD_DIM) ** -0.5 * DN_BETA),
        "attn_sinks": nrm(ks[6], (L, N_Q_HEADS), 0.5),
        "w_out": nrm(ks[7], (L, D_MODEL, D_MODEL), D_MODEL ** -0.5 * DN_BETA),
        "ln1_g": 1.0 + nrm(ks[8], (L, D_MODEL), 0.02),
        "ln1_b": nrm(ks[9], (L, D_MODEL), 0.02),
        "w_router": nrm(ks[10], (L, D_MODEL, N_EXPERTS), D_MODEL ** -0.5),
        "b_router": nrm(ks[11], (L, N_EXPERTS), 0.01),
        "w_gu": nrm(ks[12], (L, N_EXPERTS, D_MODEL, 2 * D_EXPERT), D_MODEL ** -0.5 * DN_BETA),
        "b_gu": nrm(ks[13], (L, N_EXPERTS, 2 * D_EXPERT), 0.01),
        "w_down": nrm(ks[14], (L, N_EXPERTS, D_EXPERT, D_MODEL), D_EXPERT ** -0.5 * DN_BETA),
        "b_down": nrm(ks[15], (L, N_EXPERTS, D_MODEL), 0.01),
        "w_ple_proj": nrm(ks[16], (L, D_PLE, D_MODEL), D_PLE ** -0.5 * DN_BETA),
        "w_ple_gate": nrm(ks[17], (L, D_MODEL, D_MODEL), D_MODEL ** -0.5),
        "ln2_g": 1.0 + nrm(ks[18], (L, D_MODEL), 0.02),
        "ln2_b": nrm(ks[19], (L, D_MODEL), 0.02),
    }


def reference(x, p, w_in, conv_w, w_br_conv, w_br_attn, attn_sinks, w_out, ln1_g, ln1_b,
              w_router, b_router, w_gu, b_gu, w_down, b_down, w_ple_proj, w_ple_gate,
              ln2_g, ln2_b):
    B, S, D = x.shape
    for i in range(DEPTH):
        proj = x @ w_in[i]
        b_gate, c_gate, h, q, k, v, g_conv, g_attn = jnp.split(proj, IN_SPLITS, axis=-1)
        y_conv = short_conv_mixer(b_gate, c_gate, h, conv_w[i]) @ w_br_conv[i]
        y_attn = sliding_window_attention(q, k, v, attn_sinks[i]) @ w_br_attn[i]
        merged = jax.nn.sigmoid(g_conv) * y_conv + jax.nn.sigmoid(g_attn) * y_attn
        x1 = layer_norm(DN_ALPHA * x + merged @ w_out[i], ln1_g[i], ln1_b[i])
        moe_out = routed_experts(x1, w_router[i], b_router[i], w_gu[i], b_gu[i], w_down[i], b_down[i])
        ple = jax.nn.sigmoid(x1 @ w_ple_gate[i]) * (p[i] @ w_ple_proj[i])
        x = layer_norm(DN_ALPHA * x1 + moe_out + ple, ln2_g[i], ln2_b[i])
    return x
```

```python
import numpy as np
import concourse.bass as bass
import concourse.mybir as mybir
from concourse.bass_utils import run_bass_kernel_spmd
from contextlib import ExitStack

F32 = mybir.dt.float32
BF16 = mybir.dt.bfloat16
I32 = mybir.dt.int32
AF = mybir.ActivationFunctionType
ALU = mybir.AluOpType
AX = mybir.AxisListType

NCORES = 8
D = 1024
KC = 8
NT = 2048
TG = 512
NPASS = NT // TG
HALO = 128
NE = 32
TOPK = 4
CAP = 384
SLT = [128, 128, 128]
NSL = len(SLT)
TRASH = NE * CAP
XS_ROWS = ((TRASH + 128 + 255) // 256) * 256
DPLE = 256
ALPHA = 2.0 ** 0.25
EPS = 1e-5
NEG = -30000.0
SW_LIMIT = 7.0
SW_ALPHA = 1.702
OFF_CONV = 0
OFF_GC = 3072
OFF_Q = 4096
OFF_K = 5120
OFF_V = 5632
OFF_GA = 5888
WIN_COLS = 6912
HEAD_PERM = [4 * g + j for g in range(4) for j in (0, 2, 1, 3)]


class Buf:
    __slots__ = ("name", "lw", "rd")

    def __init__(self, name):
        self.name = name
        self.lw = None
        self.rd = {}


class FW:
    def __init__(self, nc):
        self.nc = nc
        self.engs = {"pe": nc.tensor, "act": nc.scalar, "dve": nc.vector,
                     "pool": nc.gpsimd, "sp": nc.sync}
        self.semobj = {}
        self.cnt = {}
        for k in self.engs:
            self.semobj[k] = nc.alloc_semaphore("s_" + k)
            self.cnt[k] = 0
        self.seen = {k: {} for k in self.engs}

    def _wait(self, eng, need):
        for key, val in need.items():
            if self.seen[eng].get(key, 0) >= val:
                continue
            assert val <= self.cnt[key], f"wait on future event {key}:{val}>{self.cnt[key]}"
            self.engs[eng].wait_ge(self.semobj[key], val)
            self.seen[eng][key] = val

    def _deps(self, eng, r, w, selfkey=None):
        need = {}
        for b in r:
            if b.lw is not None:
                key, val = b.lw
                if key == selfkey or (key == eng and eng == "pe"):
                    continue
                need[key] = max(need.get(key, 0), val)
        for b in w:
            if b.lw is not None:
                key, val = b.lw
                if key != eng and key != selfkey:
                    need[key] = max(need.get(key, 0), val)
            for key, val in b.rd.items():
                if key != eng and key != selfkey:
                    need[key] = max(need.get(key, 0), val)
        return need

    def _record(self, ev, r, w):
        key, val = ev
        for b in r:
            if b.rd.get(key, 0) < val:
                b.rd[key] = val
        for b in w:
            b.lw = ev
            b.rd = {}

    def op(self, eng, fn, r=(), w=(), inc=True):
        self._wait(eng, self._deps(eng, r, w))
        ins = fn(self.engs[eng])
        ev = (eng, self.cnt[eng] + 1)
        if inc:
            self.cnt[eng] += 1
            ins.then_inc(self.semobj[eng], 1)
        self._record(ev, r, w)
        return ins

    def dma(self, q, key, out=None, in_=None, r=(), w=(), fn=None):
        if key not in self.semobj:
            self.semobj[key] = self.nc.alloc_semaphore("d_" + key)
            self.cnt[key] = 0
        self._wait(q, self._deps(q, r, w, selfkey=key))
        if fn is None:
            ins = self.engs[q].dma_start(out=out, in_=in_)
        else:
            ins = fn(self.engs[q])
        self.cnt[key] += 16
        ins.then_inc(self.semobj[key], 16)
        self._record((key, self.cnt[key]), r, w)
        return ins

    def barrier(self):
        need = {k: v for k, v in self.cnt.items() if v > 0}
        for e in self.engs:
            self._wait(e, dict(need))


class Ring:
    def __init__(self, items):
        self.items = items
        self.i = 0

    def next(self):
        it = self.items[self.i % len(self.items)]
        self.i += 1
        return it


def build_program(debug=False):
    nc = bass.Bass("TRN2", target_bir_lowering=False)
    fw = FW(nc)

    def din(name, shape, dt=F32):
        return nc.dram_tensor(name, list(shape), dt, kind="ExternalInput").ap()

    xin = din("xin", [NT + HALO, D])
    pin = din("pin", [NT, DPLE])
    negflag = din("negflag", [128, 1])
    w_in = din("w_in", [D, WIN_COLS])
    conv_w = din("conv_w", [3, D])
    w_brc = din("w_brc", [D, D])
    w_bra = din("w_bra", [D, D])
    sinks = din("sinks", [16])
    w_out = din("w_out", [D, D])
    ln1_g = din("ln1_g", [D]); ln1_b = din("ln1_b", [D])
    w_router = din("w_router", [D, NE]); b_router = din("b_router", [NE])
    w_gu = din("w_gu", [NE, D, 2 * D]); b_gu = din("b_gu", [NE, 2 * D])
    w_down = din("w_down", [NE, D, D]); b_down = din("b_down", [NE, D])
    w_pp = din("w_pp", [DPLE, D]); w_pg = din("w_pg", [D, D])
    ln2_g = din("ln2_g", [D]); ln2_b = din("ln2_b", [D])
    out = nc.dram_tensor("out", [NT, D], F32, kind="ExternalOutput").ap()
    if debug:
        dbg_x1 = nc.dram_tensor("dbg_x1", [NT, D], F32, kind="ExternalOutput").ap()
        dbg_idx = nc.dram_tensor("dbg_idx", [128, 16, 4], I32, kind="ExternalOutput").ap()
        dbg_w = nc.dram_tensor("dbg_w", [128, 16, 4], F32, kind="ExternalOutput").ap()
        dbg_acc = nc.dram_tensor("dbg_acc", [NT, D], F32, kind="ExternalOutput").ap()
    xs_scr = nc.dram_tensor("xs_scr", [XS_ROWS, D], BF16, kind="Internal").ap()
    ys_scr = nc.dram_tensor("ys_scr", [TRASH + 128, D], F32, kind="Internal").ap()
    acc_scr = nc.dram_tensor("acc_scr", [NT, D], F32, kind="Internal").ap()

    stack = [ExitStack()]

    def S(name, shape, dt):
        return stack[0].enter_context(nc.sbuf_tensor(name, list(shape), dt))

    def T(name, shape, dt):
        return S(name, shape, dt), Buf(name)

    banks = [(nc.alloc_psum_tensor(f"ps{i}", [128, 512], F32), Buf(f"ps{i}")) for i in range(8)]

    ident_b, B_identb = T("ident_b", [128, 128], BF16)
    ident_f, B_identf = T("ident_f", [128, 128], F32)
    Umat, B_U = T("Umat", [128, 128], F32)
    ones_f, B_ones = T("ones_f", [128, 128], F32)
    M2, B_M2 = T("M2", [128, 4, 128], BF16)
    M2f, B_M2f = T("M2f", [128, 4, 128], BF16)
    lng, B_lng = T("lng", [128, D], F32)
    lnb, B_lnb = T("lnb", [128, D], F32)
    brep, B_brep = T("brep", [128, NE], F32)
    ebase, B_ebase = T("ebase", [128, NE], F32)
    cwT, B_cwT = T("cwT", [128, KC, 3], F32)
    esink, B_esink = T("esink", [128, 16], F32)
    bguT, B_bguT = T("bguT", [128, 16, NE], F32)
    bu1T, B_bu1T = T("bu1T", [128, 8, NE], F32)
    wr, B_wr = T("wr", [128, KC, NE], F32)
    idx_all, B_idx = T("idx_all", [128, 16, TOPK], I32)
    w_all, B_wall = T("w_all", [128, 16, TOPK], F32)
    cum, B_cum = T("cum", [128, NE], F32)
    epsT, B_eps = T("epsT", [128, 1], F32)
    nflag, B_nflag = T("nflag", [128, 1], F32)

    persist_stack = stack[0]
    stack[0] = ExitStack()
    small, B_small = T("small", [32, 2 * D], F32)
    small3, B_small3 = T("small3", [3, D], F32)

    def pool_fill(t, b, val):
        fw.op("pool", lambda e: e.memset(t, val), w=[b])

    def pool_sel(t, b, pattern, op, fill, base=0, cm=1):
        fw.op("pool", lambda e: e.affine_select(out=t, in_=t, pattern=pattern, compare_op=op, fill=fill,
                                                base=base, channel_multiplier=cm), r=[b], w=[b])

    c0 = [(nflag[:], negflag[:, :], B_nflag), (lng[:], ln1_g.partition_broadcast(128), B_lng),
          (lnb[:], ln1_b.partition_broadcast(128), B_lnb), (brep[:], b_router.partition_broadcast(128), B_brep),
          (esink[:], sinks.partition_broadcast(128), B_esink),
          (wr[:], w_router.rearrange("(kc p) n -> p kc n", p=128), B_wr),
          (small[0:NE, :], b_gu[:, :], B_small), (small3[:], conv_w[:, :], B_small3)]
    for o_, i_, b_ in c0:
        fw.dma("sp", "c0", o_, i_, w=[b_])
    for o_, i_, b_ in c0:
        b_.lw = ("c0", fw.cnt["c0"])

    pool_fill(ident_b[:], B_identb, 0.0)
    pool_sel(ident_b[:], B_identb, [[-1, 128]], ALU.not_equal, 1.0)
    pool_fill(ident_f[:], B_identf, 0.0)
    pool_sel(ident_f[:], B_identf, [[-1, 128]], ALU.not_equal, 1.0)
    pool_fill(Umat[:], B_U, 0.0)
    pool_sel(Umat[:], B_U, [[-1, 128]], ALU.is_ge, 1.0)
    pool_fill(ones_f[:], B_ones, 1.0)
    pool_fill(epsT[:], B_eps, EPS)
    pool_fill(cum[:], B_cum, 0.0)
    pool_fill(M2[:], B_M2, 0.0)
    for h in range(2):
        pool_sel(M2[:, h, :], B_M2, [[-1, 128]], ALU.is_gt, NEG)
        pool_sel(M2[:, 2 + h, :], B_M2, [[1, 128]], ALU.is_ge, NEG, cm=-1)
    fw.op("pool", lambda e: e.tensor_copy(out=M2f[:], in_=M2[:]), r=[B_M2], w=[B_M2f])
    fw.op("dve", lambda e: e.tensor_scalar(out=M2f[:, 0:2, :], in0=M2f[:, 0:2, :], scalar1=nflag[:, 0:1], scalar2=None,
                                           op0=ALU.add), r=[B_M2f, B_nflag], w=[B_M2f])
    fw.op("pool", lambda e: e.iota(ebase[:], pattern=[[CAP, NE]], base=0, channel_multiplier=0,
                                   allow_small_or_imprecise_dtypes=True), w=[B_ebase])
    fw.op("act", lambda e: e.activation(out=esink[:], in_=esink[:], func=AF.Exp), r=[B_esink], w=[B_esink])
    for half in range(2):
        pb, Bp = banks[half]
        for c in range(8):
            cc = half * 8 + c
            fw.op("pe", lambda e, cc=cc, c=c, pb=pb: e.transpose(out=pb[:, c * NE:(c + 1) * NE], in_=small[0:NE, cc * 128:(cc + 1) * 128],
                                                                 identity=ident_f[0:NE, 0:NE]),
                  r=[B_small, B_identf], w=[Bp], inc=(c == 7))
        fw.op("dve", lambda e, pb=pb, half=half: e.tensor_copy(out=bguT[:, half * 8:(half + 1) * 8, :],
                                                               in_=pb[:, 0:8 * NE].rearrange("p (c n) -> p c n", n=NE)),
              r=[Bp], w=[B_bguT])
    fw.op("dve", lambda e: e.tensor_scalar(out=bu1T[:], in0=bguT[:, 8:16, :], scalar1=1.0, scalar2=None, op0=ALU.add),
          r=[B_bguT], w=[B_bu1T])
    pb, Bp = banks[2]
    for c in range(8):
        fw.op("pe", lambda e, c=c: e.transpose(out=pb[:, c * 3:(c + 1) * 3], in_=small3[0:3, c * 128:(c + 1) * 128],
                                               identity=ident_f[0:3, 0:3]),
              r=[B_small3, B_identf], w=[Bp], inc=(c == 7))
    fw.op("dve", lambda e: e.tensor_copy(out=cwT[:], in_=pb[:, 0:24].rearrange("p (c n) -> p c n", n=3)), r=[Bp], w=[B_cwT])
    fw.barrier()
    stack[0].close()
    stack[0] = ExitStack()

    bufX, B_bufX = T("bufX", [128, KC, TG + HALO], BF16)
    Bx = [Buf(f"xT{i}") for i in range(5)]
    Bo = [Buf(f"oT{i}") for i in range(4)]
    cvT = S("cvT", [128, KC, TG], BF16); B_cv = [Buf(f"cv{j}") for j in range(KC)]
    sgc = S("sgc", [128, KC, TG], BF16); B_sgc = [Buf(f"sgc{j}") for j in range(KC)]
    mrg = S("mrg", [128, KC, TG], BF16); B_mrg = [Buf(f"mrg{j}") for j in range(KC)]
    qa = S("qa", [128, KC, TG], BF16); qb = S("qb", [128, KC, TG], BF16)
    B_qa = [Buf(f"qa{j}") for j in range(KC)]
    B_qb = [Buf(f"qb{j}") for j in range(KC)]
    kT = S("kT", [128, 4, TG + HALO], BF16); B_k = [Buf(f"k{g}") for g in range(4)]
    Vt = S("Vt", [128, 5, 4, 65], BF16); B_V = [Buf(f"V{i}") for i in range(5)]
    carry = S("carry", [128, KC, 2], F32); B_carry = [Buf(f"carry{j}") for j in range(KC)]
    NWS = 5
    wring = Ring([T(f"wring{i}", [128, KC, 512], BF16) for i in range(NWS)])
    wpp, B_wpp = T("wpp", [128, 2, D], BF16)
    xbr = Ring([T(f"xb{i}", [128, D], BF16) for i in range(5)])
    xfr = Ring([T(f"xf{i}", [128, D], F32) for i in range(2)])
    csb = Ring([T(f"csb{i}", [128, TG], F32) for i in range(1)])
    ur = Ring([T(f"u{i}", [128, TG + 2], F32) for i in range(2)])
    yr = Ring([T(f"y{i}", [128, TG], F32) for i in range(2)])
    t1r = Ring([T(f"t1{i}", [128, TG], F32) for i in range(1)])
    PTr = Ring([T(f"PT{i}", [128, 512], BF16) for i in range(3)])
    obr = Ring([T(f"obf{i}", [128, D], BF16) for i in range(2)])
    zr = Ring([T(f"z{i}", [128, D], F32) for i in range(4)])
    x1br = Ring([T(f"x1b{i}", [128, D], BF16) for i in range(4)])
    x1Tbr = Ring([T(f"x1Tb{i}", [128, KC, 128], BF16) for i in range(2)])
    x1Tf, B_x1Tf = T("x1Tf", [128, KC, 128], F32)
    sgr = Ring([T(f"sg{i}", [128, D], F32) for i in range(2)])
    pbr = Ring([T(f"pb{i}", [128, DPLE], BF16) for i in range(4)])
    pTr = Ring([T(f"pT{i}", [128, 2, 128], BF16) for i in range(2)])
    Lr = Ring([T(f"L{i}", [128, NE], F32) for i in range(4)])
    top8r = Ring([T(f"top8{i}", [128, 8], F32) for i in range(4)])
    maskr = Ring([T(f"mask{i}", [128, NE], F32) for i in range(4)])
    Gt, B_Gt = T("Gt", [128, NE], F32)
    GTa = Ring([T(f"GTa{i}", [NE, 128], F32) for i in range(2)])
    bd, B_bd = T("bd", [NE, D], F32)
    fw.dma("sp", "c3", bd[:], b_down[:, :], w=[B_bd])
    dest, B_dest = T("dest", [128, NE], F32)
    oh, B_oh = T("oh", [128, TOPK, NE], F32)
    prod, B_prod = T("prod", [128, TOPK, NE], F32)
    dsel, B_dsel = T("dsel", [128, TOPK], F32)
    e4, B_e4 = T("e4", [128, TOPK], F32)
    st1, B_st1 = T("st1", [128, 8], F32)
    st2, B_st2 = T("st2", [128, 8], F32)
    stats, B_stats = T("stats", [128, 2, 6], F32)
    mv, B_mv = T("mv", [128, 2], F32)
    den, B_den = T("den", [128, 16], F32)
    rden, B_rden = T("rden", [128, 16], F32)
    chalo, B_chalo = T("chalo", [128, 2], F32)

    ps_ring = Ring(banks[0:5])
    ops = banks[5:8]

    def slot_ap(s):
        return ops[s // 7][0][:, (s % 7) * 65:(s % 7) * 65 + 65], ops[s // 7][1]

    fw.op("pool", lambda e: e.memset(qa[:], 0.0), w=B_qa)
    fw.op("pool", lambda e: e.memset(qb[:], 0.0), w=B_qb)
    fw.op("pool", lambda e: e.memset(Vt[:], 1.0), w=B_V)
    B_xsz = Buf("xs_zero")
    B_ysz = Buf("ys_zero")
    B_xs = [Buf(f"xs{t}") for t in range(16)]
    B_acc = [Buf(f"acc{t}") for t in range(16)]
    B_dbg = Buf("dbg")

    witems = []
    for p in range(NPASS):
        for j in range(KC):
            witems.append((w_in[:, OFF_CONV + j * 384: OFF_CONV + (j + 1) * 384], 384))
        for pc in range(2):
            witems.append((w_in[:, OFF_GC + pc * 512: OFF_GC + (pc + 1) * 512], 512))
        for pc in range(2):
            witems.append((w_brc[:, pc * 512:(pc + 1) * 512], 512))
        for pc in range(2):
            witems.append((w_in[:, OFF_Q + pc * 512: OFF_Q + (pc + 1) * 512], 512))
        witems.append((w_in[:, OFF_K: OFF_K + 512], 512))
        witems.append((w_in[:, OFF_V: OFF_V + 256], 256))
        for pc in range(2):
            witems.append((w_in[:, OFF_GA + pc * 512: OFF_GA + (pc + 1) * 512], 512))
        for pc in range(2):
            witems.append((w_bra[:, pc * 512:(pc + 1) * 512], 512))
        for h in range(2):
            witems.append((w_out[:, h * 512:(h + 1) * 512], 512))
        for h in range(2):
            witems.append((w_pg[:, h * 512:(h + 1) * 512], 512))
    wstate = {"issued": 0, "taken": 0, "prefix": 0, "rel": set(), "loaded": []}

    def w_prefetch():
        while wstate["issued"] < len(witems) and wstate["issued"] < wstate["prefix"] + NWS:
            src_ap, ncols = witems[wstate["issued"]]
            wt, bw = wring.next()
            fw.dma("pool", "w_" + bw.name, wt[:, :, 0:ncols], src_ap.rearrange("(kc p) n -> p kc n", p=128), w=[bw])
            wstate["loaded"].append((wt, bw))
            wstate["issued"] += 1

    def load_w():
        w_prefetch()
        assert wstate["taken"] < wstate["issued"], "weight ring exhausted"
        i_ = wstate["taken"]
        wt, bw = wstate["loaded"][i_]
        wstate["taken"] += 1
        return wt, bw, i_

    def w_release(*idx):
        for i_ in idx:
            wstate["rel"].add(i_)
        while wstate["prefix"] in wstate["rel"]:
            wstate["prefix"] += 1
        w_prefetch()

    def mm_feat(ps, Bps, wt, bw, c0, rhs_fn, rbufs, n, ncol=128, pcol0=0):
        for k in range(KC):
            fw.op("pe", lambda e, k=k: e.matmul(ps[0:ncol, pcol0:pcol0 + n], lhsT=wt[:, k, c0:c0 + ncol], rhs=rhs_fn(k),
                                                start=(k == 0), stop=(k == KC - 1)),
                  r=[bw] + rbufs, w=[Bps], inc=(k == KC - 1))

    xT = bufX
    oT = bufX
    Bx_main = Bx[1:5]

    def xT_main(k):
        return xT[:, k, HALO:HALO + TG]

    xb_pending = {}

    def step0_load(p, tiles=range(5)):
        xrow0 = p * TG
        xb_pending.setdefault(p, {})
        for i in tiles:
            xb, Bxb = xbr.next()
            fw.dma("pool", "xb_" + Bxb.name, xb[:], xin[xrow0 + i * 128: xrow0 + (i + 1) * 128, :], w=[Bxb])
            xb_pending[p][i] = (xb, Bxb)

    def step0(p):
        for i in range(5):
            xb, Bxb = xb_pending[p][i]
            ps, Bps = ps_ring.next()
            psb = ps[:].bitcast(BF16).rearrange("p (c t) -> p c t", c=KC)
            for c in range(KC):
                fw.op("pe", lambda e, c=c, psb=psb, xb=xb: e.transpose(out=psb[:, c, :], in_=xb[:, c * 128:(c + 1) * 128], identity=ident_b[:]),
                      r=[Bxb, B_identb], w=[Bps], inc=(c == KC - 1))
            fw.op("act", lambda e, i=i, psb=psb: e.activation(out=xT[:, :, i * 128:(i + 1) * 128], in_=psb, func=AF.Copy),
                  r=[Bps], w=[Bx[i]] + (Bo if i == 0 else []))

    def conv_chunk(p, j):
        if True:
            wt, bw, wi = load_w()
            ps_c, Bc = ps_ring.next()
            mm_feat(ps_c, Bc, wt, bw, 0, xT_main, Bx_main, TG)
            ps_h, Bh = ps_ring.next()
            mm_feat(ps_h, Bh, wt, bw, 128, xT_main, Bx_main, TG)
            ps_b, Bb = ps_ring.next()
            mm_feat(ps_b, Bb, wt, bw, 256, xT_main, Bx_main, TG)
            u, Bu = ur.next()
            if p == 0:
                ps_x, Bxh = ps_ring.next()
                mm_feat(ps_x, Bxh, wt, bw, 0, lambda k: xT[:, k, HALO - 2:HALO], [Bx[0]], 2, pcol0=0)
                mm_feat(ps_x, Bxh, wt, bw, 128, lambda k: xT[:, k, HALO - 2:HALO], [Bx[0]], 2, pcol0=2)
                fw.op("act", lambda e: e.activation(out=chalo[:], in_=ps_x[:, 0:2], func=AF.Copy), r=[Bxh], w=[B_chalo])
                fw.op("dve", lambda e: e.tensor_tensor(out=u[:, 0:2], in0=chalo[:], in1=ps_x[:, 2:4], op=ALU.mult),
                      r=[B_chalo, Bxh], w=[Bu])
            else:
                fw.op("pool", lambda e, j=j: e.tensor_copy(out=u[:, 0:2], in_=carry[:, j, :]), r=[B_carry[j]], w=[Bu])
            cs, Bcs = csb.next()
            fw.op("act", lambda e: e.activation(out=cs[:], in_=ps_c[:], func=AF.Copy), r=[Bc], w=[Bcs])
            fw.op("dve", lambda e: e.tensor_tensor(out=u[:, 2:TG + 2], in0=cs[:], in1=ps_h[:], op=ALU.mult),
                  r=[Bcs, Bh], w=[Bu])
            fw.op("pool", lambda e, j=j: e.tensor_copy(out=carry[:, j, :], in_=u[:, TG:TG + 2]), r=[Bu], w=[B_carry[j]])
            y, By = yr.next()
            fw.op("act", lambda e, j=j: e.activation(out=y[:], in_=u[:, 0:TG], func=AF.Copy, scale=cwT[:, j, 0:1]),
                  r=[Bu, B_cwT], w=[By])
            fw.op("dve", lambda e, j=j: e.scalar_tensor_tensor(out=y[:], in0=u[:, 1:TG + 1], scalar=cwT[:, j, 1:2], in1=y[:],
                                                               op0=ALU.mult, op1=ALU.add), r=[Bu, B_cwT, By], w=[By])
            fw.op("dve", lambda e, j=j: e.scalar_tensor_tensor(out=y[:], in0=u[:, 2:TG + 2], scalar=cwT[:, j, 2:3], in1=y[:],
                                                               op0=ALU.mult, op1=ALU.add), r=[Bu, B_cwT, By], w=[By])
            fw.op("dve", lambda e, j=j: e.tensor_tensor(out=cvT[:, j, :], in0=ps_b[:], in1=y[:], op=ALU.mult),
                  r=[Bb, By], w=[B_cv[j]])
            w_release(wi)

    def steps1to4(p):
        for pc in range(2):
            wt, bw, wi = load_w()
            for cc in range(4):
                j = pc * 4 + cc
                ps, Bps = ps_ring.next()
                mm_feat(ps, Bps, wt, bw, cc * 128, xT_main, Bx_main, TG)
                fw.op("act", lambda e, j=j, ps=ps: e.activation(out=sgc[:, j, :], in_=ps[:], func=AF.Sigmoid), r=[Bps], w=[B_sgc[j]])
            w_release(wi)
        for pc in range(2):
            wt, bw, wi = load_w()
            for cc in range(4):
                i = pc * 4 + cc
                ps, Bps = ps_ring.next()
                mm_feat(ps, Bps, wt, bw, cc * 128, lambda k: cvT[:, k, :], B_cv, TG)
                fw.op("dve", lambda e, i=i, ps=ps: e.tensor_tensor(out=mrg[:, i, :], in0=ps[:], in1=sgc[:, i, :], op=ALU.mult),
                      r=[Bps, B_sgc[i]], w=[B_mrg[i]])
            w_release(wi)
        for pc in range(2):
            wt, bw, wi = load_w()
            for cc in range(4):
                c = pc * 4 + cc
                ps, Bps = ps_ring.next()
                mm_feat(ps, Bps, wt, bw, cc * 128, xT_main, Bx_main, TG)
                fw.op("act", lambda e, c=c, ps=ps: e.activation(out=qa[0:64, c, :], in_=ps[0:64, :], func=AF.Copy), r=[Bps], w=[B_qa[c]])
                fw.op("dve", lambda e, c=c, ps=ps: e.tensor_copy(out=qb[64:128, c, :], in_=ps[64:128, :]), r=[Bps], w=[B_qb[c]])
            w_release(wi)
        wt, bw, wi = load_w()
        for g in range(4):
            if p == 0:
                ps, Bps = ps_ring.next()
                mm_feat(ps, Bps, wt, bw, g * 128, lambda k: xT[:, k, 0:HALO], [Bx[0]], HALO)
                fw.op("dve", lambda e, g=g, ps=ps: e.tensor_copy(out=kT[:, g, 0:HALO], in_=ps[:, 0:HALO]), r=[Bps], w=[B_k[g]])
            else:
                fw.op("pool", lambda e, g=g: e.tensor_copy(out=kT[:, g, 0:HALO], in_=kT[:, g, TG:TG + HALO]), r=[B_k[g]], w=[B_k[g]])
            ps, Bps = ps_ring.next()
            mm_feat(ps, Bps, wt, bw, g * 128, xT_main, Bx_main, TG)
            fw.op("dve", lambda e, g=g, ps=ps: e.tensor_copy(out=kT[:, g, HALO:HALO + TG], in_=ps[:]), r=[Bps], w=[B_k[g]])
        w_release(wi)
        wt, bw, wi = load_w()
        for i in range(5):
            if i == 0 and p > 0:
                fw.op("pool", lambda e: e.tensor_copy(out=Vt[:, 0, :, :], in_=Vt[:, 4, :, :]), r=[B_V[4]], w=[B_V[0]])
                continue
            ps, Bps = ps_ring.next()
            for k in range(KC):
                fw.op("pe", lambda e, k=k, i=i, ps=ps, wt=wt: e.matmul(ps[:, 0:256], lhsT=xT[:, k, i * 128:(i + 1) * 128], rhs=wt[:, k, 0:256],
                                                                       start=(k == 0), stop=(k == KC - 1)),
                      r=[bw, Bx[i]], w=[Bps], inc=(k == KC - 1))
            fw.op("act", lambda e, i=i, ps=ps: e.activation(out=Vt[:, i, :, 0:64], in_=ps[:, 0:256].rearrange("p (g d) -> p g d", g=4),
                                                            func=AF.Copy), r=[Bps], w=[B_V[i]])
        w_release(wi)
        sga = cvT
        B_sga = B_cv
        for pc in range(2):
            wt, bw, wi = load_w()
            for cc in range(4):
                j = pc * 4 + cc
                ps, Bps = ps_ring.next()
                mm_feat(ps, Bps, wt, bw, cc * 128, xT_main, Bx_main, TG)
                fw.op("act", lambda e, j=j, ps=ps: e.activation(out=sga[:, j, :], in_=ps[:], func=AF.Sigmoid), r=[Bps], w=[B_sga[j]])
            w_release(wi)
        if p + 1 < NPASS:
            step0_load(p + 1)
        units = [(b, g, half) for b in range(4) for g in range(4) for half in range(2)]

        def scores(b, g, half):
            first = (p == 0 and b == 0)
            msk, Bm = (M2f, B_M2f) if first else (M2, B_M2)
            qc = slice(b * 128, (b + 1) * 128)
            qsrc = qa if half == 0 else qb
            Bqs = B_qa if half == 0 else B_qb
            ps, Bps = ps_ring.next()
            for kb in range(2):
                fw.op("pe", lambda e, kb=kb: e.matmul(ps[:, kb * 256:(kb + 1) * 256], lhsT=ident_b[:],
                                                      rhs=msk[:, 2 * kb:2 * kb + 2, :].rearrange("p a b -> p (a b)"),
                                                      start=True, stop=False), r=[B_identb, Bm], w=[Bps], inc=False)
                fw.op("pe", lambda e, kb=kb: e.matmul(
                    ps[:, kb * 256:(kb + 1) * 256], lhsT=kT[:, g, (b + kb) * 128:(b + kb + 1) * 128],
                    rhs=qsrc[:, 2 * g:2 * g + 2, qc], start=False, stop=True),
                      r=[B_k[g], Bqs[2 * g], Bqs[2 * g + 1]], w=[Bps], inc=(kb == 1))
            PT, BPT = PTr.next()
            fw.op("act", lambda e: e.activation(out=PT[:], in_=ps[:], func=AF.Exp, scale=0.125), r=[Bps], w=[BPT])
            return PT, BPT

        def pv(b, g, half, PT, BPT):
            for jj in range(2):
                s_ = 4 * g + 2 * half + jj
                oap, Bop = slot_ap(s_)
                for kb in range(2):
                    fw.op("pe", lambda e, kb=kb, jj=jj: e.matmul(
                        oap, lhsT=PT[:, kb * 256 + jj * 128: kb * 256 + (jj + 1) * 128], rhs=Vt[:, b + kb, g, :],
                        start=(kb == 0), stop=(kb == 1)),
                          r=[BPT, B_V[b + kb]], w=[Bop], inc=(kb == 1))

        def finish_block(b):
            ob, Bob = obr.next()
            for bk in range(3):
                n = 7 if bk < 2 else 2
                s0 = bk * 7
                pt, Bpt = ops[bk]
                v3 = pt[:, 0:n * 65].rearrange("p (s c) -> p s c", c=65)
                fw.op("dve", lambda e, v3=v3, s0=s0, n=n: e.tensor_tensor(out=den[:, s0:s0 + n].unsqueeze(2), in0=v3[:, :, 64:65],
                                                                           in1=esink[:, s0:s0 + n].unsqueeze(2), op=ALU.add),
                      r=[Bpt, B_esink], w=[B_den])
            fw.op("dve", lambda e: e.reciprocal(out=rden[:], in_=den[:]), r=[B_den], w=[B_rden])
            for bk in range(3):
                n = 7 if bk < 2 else 2
                s0 = bk * 7
                pt, Bpt = ops[bk]
                v3 = pt[:, 0:n * 65].rearrange("p (s c) -> p s c", c=65)
                fw.op("dve", lambda e, v3=v3, s0=s0, n=n: e.tensor_tensor(
                    out=ob[:, s0 * 64:(s0 + n) * 64].rearrange("p (s d) -> p s d", d=64), in0=v3[:, :, 0:64],
                    in1=rden[:, s0:s0 + n].unsqueeze(2).to_broadcast([128, n, 64]), op=ALU.mult),
                      r=[Bpt, B_rden], w=[Bob])
            return ob, Bob

        def transpose_block(b, ob, Bob):
            ps, Bps = ps_ring.next()
            psb = ps[:].bitcast(BF16).rearrange("p (c t) -> p c t", c=KC)
            for c in range(KC):
                fw.op("pe", lambda e, c=c: e.transpose(out=psb[:, c, :], in_=ob[:, c * 128:(c + 1) * 128], identity=ident_b[:]),
                      r=[Bob, B_identb], w=[Bps], inc=(c == KC - 1))
            fw.op("act", lambda e: e.activation(out=oT[:, :, b * 128:(b + 1) * 128], in_=psb, func=AF.Copy),
                  r=[Bps], w=[Bo[b]] + (Bx if b == 0 else []))

        pend = None
        pend_tr = None
        for ui, (b, g, half) in enumerate(units):
            PT, BPT = scores(b, g, half)
            if pend is not None:
                (pb_, pg_, ph_), pPT, pBPT = pend
                pv(pb_, pg_, ph_, pPT, pBPT)
                if pg_ == 3 and ph_ == 1:
                    if pend_tr is not None:
                        transpose_block(*pend_tr)
                    pend_tr = (pb_,) + finish_block(pb_)
            pend = ((b, g, half), PT, BPT)
        (pb_, pg_, ph_), pPT, pBPT = pend
        pv(pb_, pg_, ph_, pPT, pBPT)
        if pend_tr is not None:
            transpose_block(*pend_tr)
        transpose_block(pb_, *finish_block(pb_))
        for pc in range(2):
            wt, bw, wi = load_w()
            for cc in range(4):
                i = pc * 4 + cc
                ps, Bps = ps_ring.next()
                mm_feat(ps, Bps, wt, bw, cc * 128, lambda k: oT[:, k, 0:TG], Bo, TG)
                t1, Bt1 = t1r.next()
                fw.op("dve", lambda e, i=i, ps=ps, t1=t1: e.tensor_tensor(out=t1[:], in0=ps[:], in1=sga[:, i, :], op=ALU.mult),
                      r=[Bps, B_sga[i]], w=[Bt1])
                fw.op("pool", lambda e, i=i, t1=t1: e.tensor_tensor(out=mrg[:, i, :], in0=t1[:], in1=mrg[:, i, :], op=ALU.add),
                      r=[Bt1, B_mrg[i]], w=[B_mrg[i]])
            w_release(wi)

    def step5(p, wo, wg, nxt_conv=None):
        ctx = {}

        def load_x(t):
            tok0 = p * TG + t * 128
            xf, Bxf = xfr.next()
            fw.dma("sp", "xf_" + Bxf.name, xf[:], xin[HALO + tok0: HALO + tok0 + 128, :], w=[Bxf])
            pb_, Bpb = pbr.next()
            fw.dma("pool", "pb_" + Bpb.name, pb_[:], pin[tok0:tok0 + 128, :], w=[Bpb])
            ctx[("x", t)] = (xf, Bxf, pb_, Bpb)

        def stageA(t):
            tc_ = slice(t * 128, (t + 1) * 128)
            tok0 = p * TG + t * 128
            xf, Bxf, pb_, Bpb = ctx[("x", t)]
            z, Bz = zr.next()
            for h in range(2):
                ps, Bps = ps_ring.next()
                wt, bw, _ = wo[h]
                for k in range(KC):
                    fw.op("pe", lambda e, k=k, ps=ps, wt=wt: e.matmul(ps[:, :], lhsT=mrg[:, k, tc_], rhs=wt[:, k, :],
                                                                      start=(k == 0), stop=(k == KC - 1)),
                          r=[bw, B_mrg[k]], w=[Bps], inc=(k == KC - 1))
                fw.op("dve", lambda e, h=h, ps=ps: e.scalar_tensor_tensor(
                    out=z[:, h * 512:(h + 1) * 512], in0=xf[:, h * 512:(h + 1) * 512], scalar=ALPHA, in1=ps[:], op0=ALU.mult, op1=ALU.add),
                      r=[Bps, Bxf], w=[Bz])
                fw.op("dve", lambda e, h=h: e.bn_stats(out=stats[:, h, :], in_=z[:, h * 512:(h + 1) * 512]), r=[Bz], w=[B_stats])
            fw.op("dve", lambda e: e.bn_aggr(out=mv[:], in_=stats[:].rearrange("p a b -> p (a b)")), r=[B_stats], w=[B_mv])
            fw.op("act", lambda e: e.activation(out=st1[:, 0:1], in_=mv[:, 1:2], func=AF.Sqrt, bias=epsT[:, 0:1]), r=[B_mv, B_eps], w=[B_st1])
            fw.op("dve", lambda e: e.reciprocal(out=st1[:, 1:2], in_=st1[:, 0:1]), r=[B_st1], w=[B_st1])
            fw.op("dve", lambda e: e.tensor_scalar(out=st1[:, 2:3], in0=mv[:, 0:1], scalar1=st1[:, 1:2], scalar2=-1.0,
                                                   op0=ALU.mult, op1=ALU.mult), r=[B_st1, B_mv], w=[B_st1])
            fw.op("act", lambda e: e.activation(out=z[:], in_=z[:], func=AF.Identity, scale=st1[:, 1:2], bias=st1[:, 2:3]),
                  r=[Bz, B_st1], w=[Bz])
            fw.op("dve", lambda e: e.tensor_tensor(out=z[:], in0=z[:], in1=lng[:], op=ALU.mult), r=[Bz, B_lng], w=[Bz])
            fw.op("dve", lambda e: e.tensor_tensor(out=z[:], in0=z[:], in1=lnb[:], op=ALU.add), r=[Bz, B_lnb], w=[Bz])
            if debug:
                fw.dma("sp", "dbg", dbg_x1[tok0:tok0 + 128, :], z[:], r=[Bz], w=[B_dbg])
            x1b, Bx1b = x1br.next()
            fw.op("act", lambda e: e.activation(out=x1b[:], in_=z[:], func=AF.Copy), r=[Bz], w=[Bx1b])
            ctx[("A", t)] = (z, Bz, x1b, Bx1b, pb_, Bpb)
            if t + 2 < 4:
                load_x(t + 2)

        def stageBt(t):
            x1, Bz, x1b, Bx1b, pb_, Bpb = ctx[("A", t)]
            ps, Bps = ps_ring.next()
            psb = ps[:].bitcast(BF16).rearrange("p (c t) -> p c t", c=KC)
            for c in range(KC):
                fw.op("pe", lambda e, c=c: e.transpose(out=psb[:, c, :], in_=x1b[:, c * 128:(c + 1) * 128], identity=ident_b[:]),
                      r=[Bx1b, B_identb], w=[Bps], inc=(c == KC - 1))
            x1Tb, Bx1Tb = x1Tbr.next()
            fw.op("act", lambda e: e.activation(out=x1Tb[:], in_=psb, func=AF.Copy), r=[Bps], w=[Bx1Tb])
            for hh in range(2):
                ps, Bps = ps_ring.next()
                for c in range(4):
                    cc = hh * 4 + c
                    fw.op("pe", lambda e, c=c, cc=cc, ps=ps: e.transpose(out=ps[:, c * 128:(c + 1) * 128], in_=x1[:, cc * 128:(cc + 1) * 128],
                                                                         identity=ident_f[:]),
                          r=[Bz, B_identf], w=[Bps], inc=(c == 3))
                if hh == 0:
                    fw.op("act", lambda e, ps=ps: e.activation(out=x1Tf[:, 0:4, :], in_=ps[:].rearrange("p (c t) -> p c t", c=4), func=AF.Copy),
                          r=[Bps], w=[B_x1Tf])
                else:
                    fw.op("act", lambda e, ps=ps: e.activation(out=x1Tf[:, 4:8, :], in_=ps[:].rearrange("p (c t) -> p c t", c=4), func=AF.Copy),
                          r=[Bps], w=[B_x1Tf])
            ps, Bps = ps_ring.next()
            psb2 = ps[:].bitcast(BF16)
            for c in range(2):
                fw.op("pe", lambda e, c=c: e.transpose(out=psb2[:, c * 128:(c + 1) * 128], in_=pb_[:, c * 128:(c + 1) * 128], identity=ident_b[:]),
                      r=[Bpb, B_identb], w=[Bps], inc=(c == 1))
            pT, BpT = pTr.next()
            fw.op("act", lambda e: e.activation(out=pT[:], in_=psb2[:, 0:256].rearrange("p (c t) -> p c t", c=2), func=AF.Copy),
                  r=[Bps], w=[BpT])
            ps, Bps = ps_ring.next()
            for k in range(KC):
                fw.op("pe", lambda e, k=k: e.matmul(ps[:, 0:NE], lhsT=x1Tf[:, k, :], rhs=wr[:, k, :], start=(k == 0), stop=(k == KC - 1)),
                      r=[B_x1Tf, B_wr], w=[Bps], inc=(k == KC - 1))
            L, BL = Lr.next()
            fw.op("dve", lambda e: e.tensor_tensor(out=L[:], in0=ps[:, 0:NE], in1=brep[:], op=ALU.add), r=[Bps, B_brep], w=[BL])
            top8, Bt8 = top8r.next()
            fw.op("dve", lambda e: e.max(out=top8[:], in_=L[:]), r=[BL], w=[Bt8])
            mask, Bmk = maskr.next()
            fw.op("dve", lambda e: e.tensor_scalar(out=mask[:], in0=L[:], scalar1=top8[:, 3:4], scalar2=None, op0=ALU.is_ge),
                  r=[BL, Bt8], w=[Bmk])
            ctx[("Bm", t)] = (L, BL, top8, Bt8, mask, Bmk)
            ctx[("Bt", t)] = (x1Tb, Bx1Tb, pT, BpT)

        def stageBm(t):
            Tg = p * 4 + t
            tok0 = p * TG + t * 128
            x1, Bz, x1b, Bx1b, pb_, Bpb = ctx[("A", t)]
            x1Tb, Bx1Tb, pT, BpT = ctx[("Bt", t)]
            sg, Bsg = sgr.next()
            for h in range(2):
                ps, Bps = ps_ring.next()
                wt, bw, _ = wg[h]
                for k in range(KC):
                    fw.op("pe", lambda e, k=k, ps=ps, wt=wt: e.matmul(ps[:, :], lhsT=x1Tb[:, k, :], rhs=wt[:, k, :],
                                                                      start=(k == 0), stop=(k == KC - 1)),
                          r=[bw, Bx1Tb], w=[Bps], inc=(k == KC - 1))
                fw.op("act", lambda e, h=h, ps=ps: e.activation(out=sg[:, h * 512:(h + 1) * 512], in_=ps[:], func=AF.Sigmoid),
                      r=[Bps], w=[Bsg])
                ps2, Bps2 = ps_ring.next()
                for k in range(2):
                    fw.op("pe", lambda e, k=k, ps2=ps2, h=h: e.matmul(ps2[:, :], lhsT=pT[:, k, :], rhs=wpp[:, k, h * 512:(h + 1) * 512],
                                                                      start=(k == 0), stop=(k == 1)),
                          r=[B_wpp, BpT], w=[Bps2], inc=(k == 1))
                fw.op("dve", lambda e, h=h, ps2=ps2: e.tensor_tensor(out=sg[:, h * 512:(h + 1) * 512], in0=ps2[:], in1=sg[:, h * 512:(h + 1) * 512],
                                                                     op=ALU.mult), r=[Bps2, Bsg], w=[Bsg])
            fw.op("dve", lambda e: e.scalar_tensor_tensor(out=sg[:], in0=x1[:], scalar=ALPHA, in1=sg[:], op0=ALU.mult, op1=ALU.add),
                  r=[Bz, Bsg], w=[Bsg])
            if debug:
                fw.dma("sp", "dbg", dbg_acc[tok0:tok0 + 128, :], sg[:], r=[Bsg], w=[B_dbg])
            ctx[("sg", t)] = (sg, Bsg)

        def stageC(t):
            Tg = p * 4 + t
            x1, Bz, x1b, Bx1b, pb_, Bpb = ctx[("A", t)]
            L, BL, top8, Bt8, mask, Bmk = ctx[("Bm", t)]
            ps, Bps = ps_ring.next()
            fw.op("pe", lambda e: e.matmul(ps[:, 0:NE], lhsT=Umat[:], rhs=mask[:], start=True, stop=False),
                  r=[B_U, Bmk], w=[Bps], inc=False)
            fw.op("pe", lambda e: e.matmul(ps[:, 0:NE], lhsT=ones_f[:], rhs=cum[:], start=False, stop=True),
                  r=[B_ones, B_cum], w=[Bps])
            fw.op("pool", lambda e: e.tensor_tensor(out=cum[:], in0=cum[:], in1=mask[:], op=ALU.add), r=[B_cum, Bmk], w=[B_cum])
            fw.op("dve", lambda e: e.scalar_tensor_tensor(out=dest[:], in0=ps[:, 0:NE], scalar=float(CAP - 1), in1=ebase[:], op0=ALU.min, op1=ALU.add),
                  r=[Bps, B_ebase], w=[B_dest])
            fw.op("dve", lambda e: e.tensor_tensor(out=oh[:], in0=L[:].unsqueeze(1).to_broadcast([128, TOPK, NE]),
                                                   in1=top8[:, 0:TOPK].unsqueeze(2).to_broadcast([128, TOPK, NE]), op=ALU.is_equal),
                  r=[BL, Bt8], w=[B_oh])
            fw.op("dve", lambda e: e.tensor_tensor(out=prod[:], in0=oh[:], in1=dest[:].unsqueeze(1).to_broadcast([128, TOPK, NE]), op=ALU.mult),
                  r=[B_oh, B_dest], w=[B_prod])
            fw.op("dve", lambda e: e.tensor_reduce(out=dsel[:], in_=prod[:], axis=AX.X, op=ALU.add), r=[B_prod], w=[B_dsel])
            fw.op("dve", lambda e: e.tensor_copy(out=idx_all[:, Tg, :], in_=dsel[:]), r=[B_dsel], w=[B_idx])
            fw.op("dve", lambda e: e.tensor_scalar(out=st2[:, 3:4], in0=top8[:, 0:1], scalar1=-1.0, scalar2=None, op0=ALU.mult),
                  r=[Bt8], w=[B_st2])
            fw.op("act", lambda e: e.activation(out=e4[:], in_=top8[:, 0:TOPK], func=AF.Exp, bias=st2[:, 3:4], accum_out=st2[:, 4:5]),
                  r=[Bt8, B_st2], w=[B_e4, B_st2])
            fw.op("dve", lambda e: e.reciprocal(out=st2[:, 5:6], in_=st2[:, 4:5]), r=[B_st2], w=[B_st2])
            fw.op("dve", lambda e: e.tensor_scalar(out=w_all[:, Tg, :], in0=e4[:], scalar1=st2[:, 5:6], scalar2=None, op0=ALU.mult),
                  r=[B_e4, B_st2], w=[B_wall])
            fw.op("dve", lambda e: e.tensor_tensor(out=prod[:], in0=oh[:], in1=w_all[:, Tg, :].unsqueeze(2).to_broadcast([128, TOPK, NE]), op=ALU.mult),
                  r=[B_oh, B_wall], w=[B_prod])
            fw.op("dve", lambda e: e.tensor_reduce(out=Gt[:], in_=prod[:].rearrange("p k n -> p n k"), axis=AX.X, op=ALU.add),
                  r=[B_prod], w=[B_Gt])

            for k in range(TOPK):
                fw.dma("pool", "sc_" + Bx1b.name, r=[Bx1b, B_idx, B_xsz], w=[B_xs[Tg]],
                       fn=lambda e, k=k: e.indirect_dma_start(
                           out=xs_scr[:, :], out_offset=bass.IndirectOffsetOnAxis(ap=idx_all[:, Tg, k:k + 1], axis=0),
                           in_=x1b[:], in_offset=None))

        def stageC2(t):
            Tg = p * 4 + t
            sg, Bsg = ctx[("sg", t)]
            tok0 = p * TG + t * 128
            psg_, Bpsg_ = ps_ring.next()
            fw.op("pe", lambda e: e.transpose(out=psg_[0:NE, 0:128], in_=Gt[:], identity=ident_f[:]), r=[B_Gt, B_identf], w=[Bpsg_])
            GT, BGT = GTa.next()
            fw.op("act", lambda e: e.activation(out=GT[:], in_=psg_[0:NE, 0:128], func=AF.Copy), r=[Bpsg_], w=[BGT])
            for h in range(2):
                ps2, Bps2 = ps_ring.next()
                fw.op("pe", lambda e, h=h, ps2=ps2: e.matmul(ps2[:, :], lhsT=GT[:], rhs=bd[:, h * 512:(h + 1) * 512], start=True, stop=True),
                      r=[BGT, B_bd], w=[Bps2])
                fw.op("dve", lambda e, h=h, ps2=ps2: e.tensor_tensor(out=sg[:, h * 512:(h + 1) * 512], in0=ps2[:], in1=sg[:, h * 512:(h + 1) * 512], op=ALU.add),
                      r=[Bps2, Bsg], w=[Bsg])
            fw.dma("sp", "acc_" + Bsg.name, acc_scr[tok0:tok0 + 128, :], sg[:], r=[Bsg], w=[B_acc[Tg]])
        load_x(0)
        load_x(1)
        for t in range(4):
            stageA(t)
        w_release(wo[0][2], wo[1][2])
        if nxt_conv is not None:
            for j in range(NEARLY):
                nxt_conv(j)
        stageBt(0)
        stageBt(1)
        stageBm(0)
        stageC(0)
        stageBt(2)
        stageBm(1)
        stageC2(0)
        stageC(1)
        stageBt(3)
        stageBm(2)
        stageC2(1)
        stageC(2)
        stageBm(3)
        stageC2(2)
        stageC(3)
        stageC2(3)
        w_release(wg[0][2], wg[1][2])

    fw.dma("pool", "wpp", wpp[:], w_pp.rearrange("(kc p) n -> p kc n", p=128), w=[B_wpp])
    step0_load(0)
    step0(0)
    def zero_init():
        z0, Bz0 = zr.items[0]
        fw.op("pool", lambda e: e.memset(z0[:], 0.0), w=[Bz0])
        z0b = z0[:].bitcast(BF16)
        for blk in range(XS_ROWS // 256):
            fw.dma("sp", "zi", xs_scr[blk * 256:(blk + 1) * 256, :].rearrange("(p r) d -> p (r d)", r=2), z0b,
                   r=[Bz0, B_cv[NEARLY - 1]], w=[B_xsz])
        fw.dma("sp", "zy", ys_scr[TRASH:TRASH + 128, :], z0[:], r=[Bz0], w=[B_ysz])

    NEARLY = 3
    for j in range(NEARLY):
        conv_chunk(0, j)
    zero_init()
    for p in range(NPASS):
        for j in range(NEARLY, KC):
            conv_chunk(p, j)
        steps1to4(p)
        wo = [load_w() for h in range(2)]
        wg = [load_w() for h in range(2)]
        if p + 1 < NPASS:
            step0(p + 1)
            step5(p, wo, wg, nxt_conv=lambda j, p=p: conv_chunk(p + 1, j))
        else:
            step5(p, wo, wg)

    if debug:
        fw.dma("sp", "dbg", dbg_idx[:, :, :], idx_all[:], r=[B_idx], w=[B_dbg])
        fw.dma("sp", "dbg", dbg_w[:, :, :], w_all[:], r=[B_wall], w=[B_dbg])

    fw.barrier()
    stack[0].close()
    stack[0] = ExitStack()
    ps_ring = Ring(banks)
    gur = Ring([T(f"wgu{i}", [128, KC, 2 * D], BF16) for i in range(2)])
    dnr = Ring([T(f"wdn{i}", [128, KC, D], BF16) for i in range(2)])
    xstr = Ring([T(f"xst{i}", [128, NSL, D], BF16) for i in range(2)])
    xsTr = Ring([T(f"xsT{i}", [128, KC, CAP], BF16) for i in range(2)])
    hTr = Ring([T(f"hT{i}", [128, KC, CAP], BF16) for i in range(2)])
    gr = Ring([T(f"gg{i}", [128, CAP], F32) for i in range(2)])
    sr = Ring([T(f"ss{i}", [128, CAP], F32) for i in range(2)])
    tr = Ring([T(f"tt{i}", [128, CAP], F32) for i in range(2)])
    ysr = Ring([T(f"ysb{i}", [128, NSL, D], F32) for i in range(2)])
    B_ys = [Buf(f"ys{e}") for e in range(NE)]
    NP_ = SLT[0]

    def load_expert(e_):
        wgt, Bwg = gur.next()
        wdt, Bwd = dnr.next()
        fw.dma("pool", "g_" + Bwg.name, wgt[:], w_gu[e_].rearrange("(kc p) n -> p kc n", p=128), w=[Bwg])
        fw.dma("pool", "d_" + Bwd.name, wdt[:], w_down[e_].rearrange("(kc p) n -> p kc n", p=128), w=[Bwd])
        return (wgt, Bwg, wdt, Bwd)

    def load_xs(e_):
        xst, Bxst = xstr.next()
        fw.dma("sp", "xs_" + Bxst.name, xst[0:NP_, :, :], xs_scr[e_ * CAP:(e_ + 1) * CAP, :].rearrange("(p s) d -> p s d", s=NSL),
               r=B_xs, w=[Bxst])
        return xst, Bxst

    def do_transposes(xst, Bxst):
        xsT, BxsT = xsTr.next()
        for s in range(NSL):
            ps, Bps = ps_ring.next()
            psb = ps[:].bitcast(BF16).rearrange("p (c t) -> p c t", c=KC)
            n_ = SLT[s]
            for c in range(KC):
                fw.op("pe", lambda e, c=c, s=s, psb=psb, n_=n_: e.transpose(out=psb[:, c, 0:n_], in_=xst[0:n_, s, c * 128:(c + 1) * 128], identity=ident_b[0:n_, 0:n_]),
                      r=[Bxst, B_identb], w=[Bps], inc=(c == KC - 1))
            if s % 2 == 0:
                fw.op("act", lambda e, s=s, psb=psb, n_=n_: e.activation(out=xsT[:, :, s * n_:(s + 1) * n_], in_=psb[:, :, 0:n_], func=AF.Copy), r=[Bps], w=[BxsT])
            else:
                fw.op("dve", lambda e, s=s, psb=psb, n_=n_: e.tensor_copy(out=xsT[:, :, s * n_:(s + 1) * n_], in_=psb[:, :, 0:n_]), r=[Bps], w=[BxsT])
        return xsT, BxsT

    wts = {0: load_expert(0)}
    xss = {0: load_xs(0), 1: load_xs(1)}
    cur_T = do_transposes(*xss[0])
    for e_ in range(NE):
        wgt_, Bwg_, wd_, Bwd_ = wts.pop(e_)
        Bwu_ = Bwg_
        wg_ = wgt_[:, :, 0:D]
        wu_ = wgt_[:, :, D:2 * D]
        if e_ + 1 < NE:
            wts[e_ + 1] = load_expert(e_ + 1)
        xsT, BxsT = cur_T
        hT, BhT = hTr.next()
        for j in range(KC):
            psg, Bpg = ps_ring.next()
            for k in range(KC):
                fw.op("pe", lambda e, k=k, j=j, psg=psg: e.matmul(psg[:, 0:CAP], lhsT=wg_[:, k, j * 128:(j + 1) * 128], rhs=xsT[:, k, :],
                                                                 start=(k == 0), stop=(k == KC - 1)),
                      r=[Bwg_, BxsT], w=[Bpg], inc=(k == KC - 1))
            psu, Bpu = ps_ring.next()
            for k in range(KC):
                fw.op("pe", lambda e, k=k, j=j, psu=psu: e.matmul(psu[:, 0:CAP], lhsT=wu_[:, k, j * 128:(j + 1) * 128], rhs=xsT[:, k, :],
                                                                 start=(k == 0), stop=(k == KC - 1)),
                      r=[Bwu_, BxsT], w=[Bpu], inc=(k == KC - 1))
            gg, Bgg = gr.next()
            ss, Bss = sr.next()
            tt, Btt = tr.next()
            fw.op("dve", lambda e, j=j, psg=psg, gg=gg: e.tensor_scalar(out=gg[:], in0=psg[:, 0:CAP], scalar1=bguT[:, j, e_:e_ + 1], scalar2=SW_LIMIT,
                                                                     op0=ALU.add, op1=ALU.min), r=[Bpg, B_bguT], w=[Bgg])
            fw.op("act", lambda e, gg=gg, ss=ss: e.activation(out=ss[:], in_=gg[:], func=AF.Sigmoid, scale=SW_ALPHA), r=[Bgg], w=[Bss])
            fw.op("dve", lambda e, j=j, psu=psu, tt=tt: e.tensor_scalar(out=tt[:], in0=psu[:, 0:CAP], scalar1=bu1T[:, j, e_:e_ + 1], scalar2=SW_LIMIT + 1.0,
                                                                     op0=ALU.add, op1=ALU.min), r=[Bpu, B_bu1T], w=[Btt])
            fw.op("dve", lambda e, gg=gg, ss=ss: e.tensor_tensor(out=ss[:], in0=gg[:], in1=ss[:], op=ALU.mult), r=[Bgg, Bss], w=[Bss])
            fw.op("dve", lambda e, j=j, tt=tt, ss=ss: e.scalar_tensor_tensor(out=hT[:, j, :], in0=tt[:], scalar=1.0 - SW_LIMIT, in1=ss[:],
                                                                           op0=ALU.max, op1=ALU.mult), r=[Btt, Bss], w=[BhT])
        if e_ + 1 < NE:
            cur_T = do_transposes(*xss.pop(e_ + 1))
            if e_ + 2 < NE:
                xss[e_ + 2] = load_xs(e_ + 2)
        ysb, Bysb = ysr.next()
        for s in range(NSL):
            n_ = SLT[s]
            for h in range(2):
                ps, Bps = ps_ring.next()
                for k in range(KC):
                    fw.op("pe", lambda e, k=k, s=s, h=h, ps=ps, n_=n_: e.matmul(ps[0:n_, :], lhsT=hT[:, k, s * n_:(s + 1) * n_], rhs=wd_[:, k, h * 512:(h + 1) * 512],
                                                                               start=(k == 0), stop=(k == KC - 1)),
                          r=[Bwd_, BhT], w=[Bps], inc=(k == KC - 1))
                fw.op("act", lambda e, s=s, h=h, ps=ps, n_=n_: e.activation(out=ysb[0:n_, s, h * 512:(h + 1) * 512], in_=ps[0:n_, :], func=AF.Copy), r=[Bps], w=[Bysb])
        fw.dma("act", "ys_" + Bysb.name, ys_scr[e_ * CAP:(e_ + 1) * CAP, :].rearrange("(p s) d -> p s d", s=NSL), ysb[0:NP_, :, :],
               r=[Bysb], w=[B_ys[e_]])

    fw.barrier()
    stack[0].close()
    stack[0] = ExitStack()
    accr = Ring([T(f"acc{i}", [128, D], F32) for i in range(6)])
    gkr = Ring([T(f"gk{i}", [128, D], F32) for i in range(24)])
    fw.dma("sp", "c2", lng[:], ln2_g.partition_broadcast(128), w=[B_lng])
    fw.dma("sp", "c2", lnb[:], ln2_b.partition_broadcast(128), w=[B_lnb])
    for b_ in (B_lng, B_lnb):
        b_.lw = ("c2", fw.cnt["c2"])
    B_out = [Buf(f"out{t}") for t in range(16)]
    st3r = Ring([T(f"st3_{i}", [128, 8], F32) for i in range(3)])
    mv3r = Ring([T(f"mv3_{i}", [128, 2], F32) for i in range(3)])
    stats3r = Ring([T(f"stats3_{i}", [128, 2, 6], F32) for i in range(3)])
    pre = {}

    def prefetch_c(Tg):
        tok0 = Tg * 128
        acc, Bacc = accr.next()
        fw.dma("sp", "la_" + Bacc.name, acc[:], acc_scr[tok0:tok0 + 128, :], r=[B_acc[Tg]], w=[Bacc])
        gks = []
        for k in range(TOPK):
            gk, Bgk = gkr.next()
            fw.dma("pool", "ga_" + Bgk.name, r=B_ys + [B_ysz, B_idx], w=[Bgk],
                   fn=lambda e, k=k, gk=gk: e.indirect_dma_start(
                       out=gk[:], out_offset=None, in_=ys_scr[:, :],
                       in_offset=bass.IndirectOffsetOnAxis(ap=idx_all[:, Tg, k:k + 1], axis=0)))
            gks.append((gk, Bgk))
        pre[Tg] = (acc, Bacc, gks)

    PFD = 4
    for Tg in range(PFD):
        prefetch_c(Tg)

    def q1(Tg):
        if Tg + PFD < 16:
            prefetch_c(Tg + PFD)
        acc, Bacc, gks = pre[Tg]
        for k in range(TOPK):
            gk, Bgk = gks[k]
            fw.op("dve", lambda e, k=k, gk=gk: e.scalar_tensor_tensor(out=acc[:], in0=gk[:], scalar=w_all[:, Tg, k:k + 1], in1=acc[:],
                                                                     op0=ALU.mult, op1=ALU.add), r=[Bgk, B_wall, Bacc], w=[Bacc])
        st, Bst = st3r.next()
        mvx, Bmvx = mv3r.next()
        sts, Bsts = stats3r.next()
        for h in range(2):
            fw.op("dve", lambda e, h=h: e.bn_stats(out=sts[:, h, :], in_=acc[:, h * 512:(h + 1) * 512]), r=[Bacc], w=[Bsts])
        fw.op("dve", lambda e: e.bn_aggr(out=mvx[:], in_=sts[:].rearrange("p a b -> p (a b)")), r=[Bsts], w=[Bmvx])
        fw.op("act", lambda e: e.activation(out=st[:, 0:1], in_=mvx[:, 1:2], func=AF.Sqrt, bias=epsT[:, 0:1]), r=[Bmvx, B_eps], w=[Bst])
        fw.op("dve", lambda e: e.reciprocal(out=st[:, 1:2], in_=st[:, 0:1]), r=[Bst], w=[Bst])
        fw.op("dve", lambda e: e.tensor_scalar(out=st[:, 2:3], in0=mvx[:, 0:1], scalar1=st[:, 1:2], scalar2=-1.0, op0=ALU.mult, op1=ALU.mult),
              r=[Bst, Bmvx], w=[Bst])
        fw.op("act", lambda e: e.activation(out=acc[:], in_=acc[:], func=AF.Identity, scale=st[:, 1:2], bias=st[:, 2:3]),
              r=[Bacc, Bst], w=[Bacc])
        fw.op("pool", lambda e: e.tensor_tensor(out=acc[:], in0=acc[:], in1=lng[:], op=ALU.mult), r=[Bacc, B_lng], w=[Bacc])

    def q2(Tg):
        tok0 = Tg * 128
        acc, Bacc, gks = pre.pop(Tg)
        fw.op("dve", lambda e: e.tensor_tensor(out=acc[:], in0=acc[:], in1=lnb[:], op=ALU.add), r=[Bacc, B_lnb], w=[Bacc])
        fw.dma("act", "out_" + Bacc.name, out[tok0:tok0 + 128, :], acc[:], r=[Bacc], w=[B_out[Tg]])

    q1(0)
    for Tg in range(16):
        if Tg + 1 < 16:
            q1(Tg + 1)
        q2(Tg)
    fw.barrier()
    stack[0].close()
    persist_stack.close()
    return nc


def _prep_inputs(inp):
    f = lambda a: np.ascontiguousarray(np.asarray(a, dtype=np.float32))
    x = f(inp["x"]); p = f(inp["p"])[0]
    w_in = f(inp["w_in"])[0]
    Bc, Cc, Hc = w_in[:, 0:1024], w_in[:, 1024:2048], w_in[:, 2048:3072]
    Q, K, V = w_in[:, 3072:4096], w_in[:, 4096:4352], w_in[:, 4352:4608]
    GC, GA = w_in[:, 4608:5632], w_in[:, 5632:6656]
    cols = []
    for j in range(8):
        sl = slice(j * 128, (j + 1) * 128)
        cols += [Cc[:, sl], Hc[:, sl], Bc[:, sl]]
    cols.append(GC); cols.append(Q)
    for g in range(4):
        cols += [K[:, g * 64:(g + 1) * 64], K[:, g * 64:(g + 1) * 64]]
    cols.append(V); cols.append(GA)
    w_in_p = np.ascontiguousarray(np.concatenate(cols, axis=1))
    assert w_in_p.shape == (D, WIN_COLS)
    rows = np.concatenate([np.arange(h * 64, (h + 1) * 64) for h in HEAD_PERM])
    shared = {
        "w_in": w_in_p,
        "conv_w": f(inp["conv_w"])[0],
        "w_brc": f(inp["w_br_conv"])[0],
        "w_bra": np.ascontiguousarray(f(inp["w_br_attn"])[0][rows]),
        "sinks": np.ascontiguousarray(f(inp["attn_sinks"])[0][HEAD_PERM]),
        "w_out": f(inp["w_out"])[0],
        "ln1_g": f(inp["ln1_g"])[0], "ln1_b": f(inp["ln1_b"])[0],
        "w_router": f(inp["w_router"])[0], "b_router": f(inp["b_router"])[0],
        "w_gu": f(inp["w_gu"])[0], "b_gu": f(inp["b_gu"])[0],
        "w_down": f(inp["w_down"])[0], "b_down": f(inp["b_down"])[0],
        "w_pp": f(inp["w_ple_proj"])[0], "w_pg": f(inp["w_ple_gate"])[0],
        "ln2_g": f(inp["ln2_g"])[0], "ln2_b": f(inp["ln2_b"])[0],
    }
    in_maps = []
    for c in range(NCORES):
        b = c // 4
        s0 = (c % 4) * NT
        halo = x[b, s0 - HALO:s0] if s0 > 0 else np.zeros((HALO, D), np.float32)
        m = dict(shared)
        m["xin"] = np.ascontiguousarray(np.concatenate([halo, x[b, s0:s0 + NT]], axis=0))
        m["pin"] = np.ascontiguousarray(p[b, s0:s0 + NT])
        m["negflag"] = np.full((128, 1), NEG if s0 == 0 else 0.0, np.float32)
        in_maps.append(m)
    return in_maps


def kernel(**inputs):
    in_maps = _prep_inputs(inputs)
    nc = build_program(debug=False)
    res = run_bass_kernel_spmd(nc, in_maps, core_ids=list(range(NCORES)))
    outs = [np.asarray(r["out"], dtype=np.float32) for r in res.results]
    full = np.stack([np.concatenate(outs[0:4], axis=0), np.concatenate(outs[4:8], axis=0)], axis=0)
    return full.astype(np.float32)
```

```python
import numpy as np
import concourse.bass as bass
import concourse.mybir as mybir
from concourse.bass_utils import run_bass_kernel_spmd
from contextlib import ExitStack

F32 = mybir.dt.float32
BF16 = mybir.dt.bfloat16
I32 = mybir.dt.int32
AF = mybir.ActivationFunctionType
ALU = mybir.AluOpType
AX = mybir.AxisListType

NCORES = 8
D = 1024
KC = 8
NT = 2048
TG = 512
NPASS = NT // TG
HALO = 128
NE = 32
TOPK = 4
CAP = 384
SLT = [128, 128, 128]
NSL = len(SLT)
TRASH = NE * CAP
XS_ROWS = ((TRASH + 128 + 255) // 256) * 256
DPLE = 256
ALPHA = 2.0 ** 0.25
EPS = 1e-5
NEG = -30000.0
SW_LIMIT = 7.0
SW_ALPHA = 1.702
OFF_CONV = 0
OFF_GC = 3072
OFF_Q = 4096
OFF_K = 5120
OFF_V = 5632
OFF_GA = 5888
WIN_COLS = 6912
HEAD_PERM = [4 * g + j for g in range(4) for j in (0, 2, 1, 3)]


class Buf:
    __slots__ = ("name", "lw", "rd")

    def __init__(self, name):
        self.name = name
        self.lw = None
        self.rd = {}


class FW:
    def __init__(self, nc):
        self.nc = nc
        self.engs = {"pe": nc.tensor, "act": nc.scalar, "dve": nc.vector,
                     "pool": nc.gpsimd, "sp": nc.sync}
        self.semobj = {}
        self.cnt = {}
        for k in self.engs:
            self.semobj[k] = nc.alloc_semaphore("s_" + k)
            self.cnt[k] = 0
        self.seen = {k: {} for k in self.engs}

    def _wait(self, eng, need):
        for key, val in need.items():
            if self.seen[eng].get(key, 0) >= val:
                continue
            assert val <= self.cnt[key], f"wait on future event {key}:{val}>{self.cnt[key]}"
            self.engs[eng].wait_ge(self.semobj[key], val)
            self.seen[eng][key] = val

    def _deps(self, eng, r, w, selfkey=None):
        need = {}
        for b in r:
            if b.lw is not None:
                key, val = b.lw
                if key == selfkey or (key == eng and eng == "pe"):
                    continue
                need[key] = max(need.get(key, 0), val)
        for b in w:
            if b.lw is not None:
                key, val = b.lw
                if key != eng and key != selfkey:
                    need[key] = max(need.get(key, 0), val)
            for key, val in b.rd.items():
                if key != eng and key != selfkey:
                    need[key] = max(need.get(key, 0), val)
        return need

    def _record(self, ev, r, w):
        key, val = ev
        for b in r:
            if b.rd.get(key, 0) < val:
                b.rd[key] = val
        for b in w:
            b.lw = ev
            b.rd = {}

    def op(self, eng, fn, r=(), w=(), inc=True):
        self._wait(eng, self._deps(eng, r, w))
        ins = fn(self.engs[eng])
        ev = (eng, self.cnt[eng] + 1)
        if inc:
            self.cnt[eng] += 1
            ins.then_inc(self.semobj[eng], 1)
        self._record(ev, r, w)
        return ins

    def dma(self, q, key, out=None, in_=None, r=(), w=(), fn=None):
        if key not in self.semobj:
            self.semobj[key] = self.nc.alloc_semaphore("d_" + key)
            self.cnt[key] = 0
        self._wait(q, self._deps(q, r, w, selfkey=key))
        if fn is None:
            ins = self.engs[q].dma_start(out=out, in_=in_)
        else:
            ins = fn(self.engs[q])
        self.cnt[key] += 16
        ins.then_inc(self.semobj[key], 16)
        self._record((key, self.cnt[key]), r, w)
        return ins

    def barrier(self):
        need = {k: v for k, v in self.cnt.items() if v > 0}
        for e in self.engs:
            self._wait(e, dict(need))


class Ring:
    def __init__(self, items):
        self.items = items
        self.i = 0

    def next(self):
        it = self.items[self.i % len(self.items)]
        self.i += 1
        return it


def build_program(debug=False):
    nc = bass.Bass("TRN2", target_bir_lowering=False)
    fw = FW(nc)

    def din(name, shape, dt=F32):
        return nc.dram_tensor(name, list(shape), dt, kind="ExternalInput").ap()

    xin = din("xin", [NT + HALO, D])
    pin = din("pin", [NT, DPLE])
    negflag = din("negflag", [128, 1])
    w_in = din("w_in", [D, WIN_COLS])
    conv_w = din("conv_w", [3, D])
    w_brc = din("w_brc", [D, D])
    w_bra = din("w_bra", [D, D])
    sinks = din("sinks", [16])
    w_out = din("w_out", [D, D])
    ln1_g = din("ln1_g", [D]); ln1_b = din("ln1_b", [D])
    w_router = din("w_router", [D, NE]); b_router = din("b_router", [NE])
    w_gu = din("w_gu", [NE, D, 2 * D]); b_gu = din("b_gu", [NE, 2 * D])
    w_down = din("w_down", [NE, D, D]); b_down = din("b_down", [NE, D])
    w_pp = din("w_pp", [DPLE, D]); w_pg = din("w_pg", [D, D])
    ln2_g = din("ln2_g", [D]); ln2_b = din("ln2_b", [D])
    out = nc.dram_tensor("out", [NT, D], F32, kind="ExternalOutput").ap()
    if debug:
        dbg_x1 = nc.dram_tensor("dbg_x1", [NT, D], F32, kind="ExternalOutput").ap()
        dbg_idx = nc.dram_tensor("dbg_idx", [128, 16, 4], I32, kind="ExternalOutput").ap()
        dbg_w = nc.dram_tensor("dbg_w", [128, 16, 4], F32, kind="ExternalOutput").ap()
        dbg_acc = nc.dram_tensor("dbg_acc", [NT, D], F32, kind="ExternalOutput").ap()
    xs_scr = nc.dram_tensor("xs_scr", [XS_ROWS, D], BF16, kind="Internal").ap()
    ys_scr = nc.dram_tensor("ys_scr", [TRASH + 128, D], F32, kind="Internal").ap()
    acc_scr = nc.dram_tensor("acc_scr", [NT, D], F32, kind="Internal").ap()

    stack = [ExitStack()]

    def S(name, shape, dt):
        return stack[0].enter_context(nc.sbuf_tensor(name, list(shape), dt))

    def T(name, shape, dt):
        return S(name, shape, dt), Buf(name)

    banks = [(nc.alloc_psum_tensor(f"ps{i}", [128, 512], F32), Buf(f"ps{i}")) for i in range(8)]

    ident_b, B_identb = T("ident_b", [128, 128], BF16)
    ident_f, B_identf = T("ident_f", [128, 128], F32)
    Umat, B_U = T("Umat", [128, 128], F32)
    ones_f, B_ones = T("ones_f", [128, 128], F32)
    M2, B_M2 = T("M2", [128, 4, 128], BF16)
    M2f, B_M2f = T("M2f", [128, 4, 128], BF16)
    lng, B_lng = T("lng", [128, D], F32)
    lnb, B_lnb = T("lnb", [128, D], F32)
    brep, B_brep = T("brep", [128, NE], F32)
    ebase, B_ebase = T("ebase", [128, NE], F32)
    cwT, B_cwT = T("cwT", [128, KC, 3], F32)
    esink, B_esink = T("esink", [128, 16], F32)
    bguT, B_bguT = T("bguT", [128, 16, NE], F32)
    bu1T, B_bu1T = T("bu1T", [128, 8, NE], F32)
    wr, B_wr = T("wr", [128, KC, NE], F32)
    idx_all, B_idx = T("idx_all", [128, 16, TOPK], I32)
    w_all, B_wall = T("w_all", [128, 16, TOPK], F32)
    cum, B_cum = T("cum", [128, NE], F32)
    epsT, B_eps = T("epsT", [128, 1], F32)
    nflag, B_nflag = T("nflag", [128, 1], F32)

    persist_stack = stack[0]
    stack[0] = ExitStack()
    small, B_small = T("small", [32, 2 * D], F32)
    small3, B_small3 = T("small3", [3, D], F32)

    def pool_fill(t, b, val):
        fw.op("pool", lambda e: e.memset(t, val), w=[b])

    def pool_sel(t, b, pattern, op, fill, base=0, cm=1):
        fw.op("pool", lambda e: e.affine_select(out=t, in_=t, pattern=pattern, compare_op=op, fill=fill,
                                                base=base, channel_multiplier=cm), r=[b], w=[b])

    c0 = [(nflag[:], negflag[:, :], B_nflag), (lng[:], ln1_g.partition_broadcast(128), B_lng),
          (lnb[:], ln1_b.partition_broadcast(128), B_lnb), (brep[:], b_router.partition_broadcast(128), B_brep),
          (esink[:], sinks.partition_broadcast(128), B_esink),
          (wr[:], w_router.rearrange("(kc p) n -> p kc n", p=128), B_wr),
          (small[0:NE, :], b_gu[:, :], B_small), (small3[:], conv_w[:, :], B_small3)]
    for o_, i_, b_ in c0:
        fw.dma("sp", "c0", o_, i_, w=[b_])
    for o_, i_, b_ in c0:
        b_.lw = ("c0", fw.cnt["c0"])

    pool_fill(ident_b[:], B_identb, 0.0)
    pool_sel(ident_b[:], B_identb, [[-1, 128]], ALU.not_equal, 1.0)
    pool_fill(ident_f[:], B_identf, 0.0)
    pool_sel(ident_f[:], B_identf, [[-1, 128]], ALU.not_equal, 1.0)
    pool_fill(Umat[:], B_U, 0.0)
    pool_sel(Umat[:], B_U, [[-1, 128]], ALU.is_ge, 1.0)
    pool_fill(ones_f[:], B_ones, 1.0)
    pool_fill(epsT[:], B_eps, EPS)
    pool_fill(cum[:], B_cum, 0.0)
    pool_fill(M2[:], B_M2, 0.0)
    for h in range(2):
        pool_sel(M2[:, h, :], B_M2, [[-1, 128]], ALU.is_gt, NEG)
        pool_sel(M2[:, 2 + h, :], B_M2, [[1, 128]], ALU.is_ge, NEG, cm=-1)
    fw.op("pool", lambda e: e.tensor_copy(out=M2f[:], in_=M2[:]), r=[B_M2], w=[B_M2f])
    fw.op("dve", lambda e: e.tensor_scalar(out=M2f[:, 0:2, :], in0=M2f[:, 0:2, :], scalar1=nflag[:, 0:1], scalar2=None,
                                           op0=ALU.add), r=[B_M2f, B_nflag], w=[B_M2f])
    fw.op("pool", lambda e: e.iota(ebase[:], pattern=[[CAP, NE]], base=0, channel_multiplier=0,
                                   allow_small_or_imprecise_dtypes=True), w=[B_ebase])
    fw.op("act", lambda e: e.activation(out=esink[:], in_=esink[:], func=AF.Exp), r=[B_esink], w=[B_esink])
    for half in range(2):
        pb, Bp = banks[half]
        for c in range(8):
            cc = half * 8 + c
            fw.op("pe", lambda e, cc=cc, c=c, pb=pb: e.transpose(out=pb[:, c * NE:(c + 1) * NE], in_=small[0:NE, cc * 128:(cc + 1) * 128],
                                                                 identity=ident_f[0:NE, 0:NE]),
                  r=[B_small, B_identf], w=[Bp], inc=(c == 7))
        fw.op("dve", lambda e, pb=pb, half=half: e.tensor_copy(out=bguT[:, half * 8:(half + 1) * 8, :],
                                                               in_=pb[:, 0:8 * NE].rearrange("p (c n) -> p c n", n=NE)),
              r=[Bp], w=[B_bguT])
    fw.op("dve", lambda e: e.tensor_scalar(out=bu1T[:], in0=bguT[:, 8:16, :], scalar1=1.0, scalar2=None, op0=ALU.add),
          r=[B_bguT], w=[B_bu1T])
    pb, Bp = banks[2]
    for c in range(8):
        fw.op("pe", lambda e, c=c: e.transpose(out=pb[:, c * 3:(c + 1) * 3], in_=small3[0:3, c * 128:(c + 1) * 128],
                                               identity=ident_f[0:3, 0:3]),
              r=[B_small3, B_identf], w=[Bp], inc=(c == 7))
    fw.op("dve", lambda e: e.tensor_copy(out=cwT[:], in_=pb[:, 0:24].rearrange("p (c n) -> p c n", n=3)), r=[Bp], w=[B_cwT])
    fw.barrier()
    stack[0].close()
    stack[0] = ExitStack()

    bufX, B_bufX = T("bufX", [128, KC, TG + HALO], BF16)
    Bx = [Buf(f"xT{i}") for i in range(5)]
    Bo = [Buf(f"oT{i}") for i in range(4)]
    cvT = S("cvT", [128, KC, TG], BF16); B_cv = [Buf(f"cv{j}") for j in range(KC)]
    sgc = S("sgc", [128, KC, TG], BF16); B_sgc = [Buf(f"sgc{j}") for j in range(KC)]
    mrg = S("mrg", [128, KC, TG], BF16); B_mrg = [Buf(f"mrg{j}") for j in range(KC)]
    qa = S("qa", [128, KC, TG], BF16); qb = S("qb", [128, KC, TG], BF16)
    B_qa = [Buf(f"qa{j}") for j in range(KC)]
    B_qb = [Buf(f"qb{j}") for j in range(KC)]
    kT = S("kT", [128, 4, TG + HALO], BF16); B_k = [Buf(f"k{g}") for g in range(4)]
    Vt = S("Vt", [128, 5, 4, 65], BF16); B_V = [Buf(f"V{i}") for i in range(5)]
    carry = S("carry", [128, KC, 2], F32); B_carry = [Buf(f"carry{j}") for j in range(KC)]
    NWS = 5
    wring = Ring([T(f"wring{i}", [128, KC, 512], BF16) for i in range(NWS)])
    wpp, B_wpp = T("wpp", [128, 2, D], BF16)
    xbr = Ring([T(f"xb{i}", [128, D], BF16) for i in range(5)])
    xfr = Ring([T(f"xf{i}", [128, D], F32) for i in range(2)])
    csb = Ring([T(f"csb{i}", [128, TG], F32) for i in range(1)])
    ur = Ring([T(f"u{i}", [128, TG + 2], F32) for i in range(2)])
    yr = Ring([T(f"y{i}", [128, TG], F32) for i in range(2)])
    t1r = Ring([T(f"t1{i}", [128, TG], F32) for i in range(1)])
    PTr = Ring([T(f"PT{i}", [128, 512], BF16) for i in range(3)])
    obr = Ring([T(f"obf{i}", [128, D], BF16) for i in range(2)])
    zr = Ring([T(f"z{i}", [128, D], F32) for i in range(4)])
    x1br = Ring([T(f"x1b{i}", [128, D], BF16) for i in range(4)])
    x1Tbr = Ring([T(f"x1Tb{i}", [128, KC, 128], BF16) for i in range(2)])
    x1Tf, B_x1Tf = T("x1Tf", [128, KC, 128], F32)
    sgr = Ring([T(f"sg{i}", [128, D], F32) for i in range(2)])
    pbr = Ring([T(f"pb{i}", [128, DPLE], BF16) for i in range(4)])
    pTr = Ring([T(f"pT{i}", [128, 2, 128], BF16) for i in range(2)])
    Lr = Ring([T(f"L{i}", [128, NE], F32) for i in range(4)])
    top8r = Ring([T(f"top8{i}", [128, 8], F32) for i in range(4)])
    maskr = Ring([T(f"mask{i}", [128, NE], F32) for i in range(4)])
    Gt, B_Gt = T("Gt", [128, NE], F32)
    GTa = Ring([T(f"GTa{i}", [NE, 128], F32) for i in range(2)])
    bd, B_bd = T("bd", [NE, D], F32)
    fw.dma("sp", "c3", bd[:], b_down[:, :], w=[B_bd])
    dest, B_dest = T("dest", [128, NE], F32)
    oh, B_oh = T("oh", [128, TOPK, NE], F32)
    prod, B_prod = T("prod", [128, TOPK, NE], F32)
    dsel, B_dsel = T("dsel", [128, TOPK], F32)
    e4, B_e4 = T("e4", [128, TOPK], F32)
    st1, B_st1 = T("st1", [128, 8], F32)
    st2, B_st2 = T("st2", [128, 8], F32)
    stats, B_stats = T("stats", [128, 2, 6], F32)
    mv, B_mv = T("mv", [128, 2], F32)
    den, B_den = T("den", [128, 16], F32)
    rden, B_rden = T("rden", [128, 16], F32)
    chalo, B_chalo = T("chalo", [128, 2], F32)

    ps_ring = Ring(banks[0:5])
    ops = banks[5:8]

    def slot_ap(s):
        return ops[s // 7][0][:, (s % 7) * 65:(s % 7) * 65 + 65], ops[s // 7][1]

    fw.op("pool", lambda e: e.memset(qa[:], 0.0), w=B_qa)
    fw.op("pool", lambda e: e.memset(qb[:], 0.0), w=B_qb)
    fw.op("pool", lambda e: e.memset(Vt[:], 1.0), w=B_V)
    B_xsz = Buf("xs_zero")
    B_ysz = Buf("ys_zero")
    B_xs = [Buf(f"xs{t}") for t in range(16)]
    B_acc = [Buf(f"acc{t}") for t in range(16)]
    B_dbg = Buf("dbg")

    witems = []
    for p in range(NPASS):
        for j in range(KC):
            witems.append((w_in[:, OFF_CONV + j * 384: OFF_CONV + (j + 1) * 384], 384))
        for pc in range(2):
            witems.append((w_in[:, OFF_GC + pc * 512: OFF_GC + (pc + 1) * 512], 512))
        for pc in range(2):
            witems.append((w_brc[:, pc * 512:(pc + 1) * 512], 512))
        for pc in range(2):
            witems.append((w_in[:, OFF_Q + pc * 512: OFF_Q + (pc + 1) * 512], 512))
        witems.append((w_in[:, OFF_K: OFF_K + 512], 512))
        witems.append((w_in[:, OFF_V: OFF_V + 256], 256))
        for pc in range(2):
            witems.append((w_in[:, OFF_GA + pc * 512: OFF_GA + (pc + 1) * 512], 512))
        for pc in range(2):
            witems.append((w_bra[:, pc * 512:(pc + 1) * 512], 512))
        for h in range(2):
            witems.append((w_out[:, h * 512:(h + 1) * 512], 512))
        for h in range(2):
            witems.append((w_pg[:, h * 512:(h + 1) * 512], 512))
    wstate = {"issued": 0, "taken": 0, "prefix": 0, "rel": set(), "loaded": []}

    def w_prefetch():
        while wstate["issued"] < len(witems) and wstate["issued"] < wstate["prefix"] + NWS:
            src_ap, ncols = witems[wstate["issued"]]
            wt, bw = wring.next()
            fw.dma("pool", "w_" + bw.name, wt[:, :, 0:ncols], src_ap.rearrange("(kc p) n -> p kc n", p=128), w=[bw])
            wstate["loaded"].append((wt, bw))
            wstate["issued"] += 1

    def load_w():
        w_prefetch()
        assert wstate["taken"] < wstate["issued"], "weight ring exhausted"
        i_ = wstate["taken"]
        wt, bw = wstate["loaded"][i_]
        wstate["taken"] += 1
        return wt, bw, i_

    def w_release(*idx):
        for i_ in idx:
            wstate["rel"].add(i_)
        while wstate["prefix"] in wstate["rel"]:
            wstate["prefix"] += 1
        w_prefetch()

    def mm_feat(ps, Bps, wt, bw, c0, rhs_fn, rbufs, n, ncol=128, pcol0=0):
        for k in range(KC):
            fw.op("pe", lambda e, k=k: e.matmul(ps[0:ncol, pcol0:pcol0 + n], lhsT=wt[:, k, c0:c0 + ncol], rhs=rhs_fn(k),
                                                start=(k == 0), stop=(k == KC - 1)),
                  r=[bw] + rbufs, w=[Bps], inc=(k == KC - 1))

    xT = bufX
    oT = bufX
    Bx_main = Bx[1:5]

    def xT_main(k):
        return xT[:, k, HALO:HALO + TG]

    xb_pending = {}

    def step0_load(p, tiles=range(5)):
        xrow0 = p * TG
        xb_pending.setdefault(p, {})
        for i in tiles:
            xb, Bxb = xbr.next()
            fw.dma("pool", "xb_" + Bxb.name, xb[:], xin[xrow0 + i * 128: xrow0 + (i + 1) * 128, :], w=[Bxb])
            xb_pending[p][i] = (xb, Bxb)

    def step0(p):
        for i in range(5):
            xb, Bxb = xb_pending[p][i]
            ps, Bps = ps_ring.next()
            psb = ps[:].bitcast(BF16).rearrange("p (c t) -> p c t", c=KC)
            for c in range(KC):
                fw.op("pe", lambda e, c=c, psb=psb, xb=xb: e.transpose(out=psb[:, c, :], in_=xb[:, c * 128:(c + 1) * 128], identity=ident_b[:]),
                      r=[Bxb, B_identb], w=[Bps], inc=(c == KC - 1))
            fw.op("act", lambda e, i=i, psb=psb: e.activation(out=xT[:, :, i * 128:(i + 1) * 128], in_=psb, func=AF.Copy),
                  r=[Bps], w=[Bx[i]] + (Bo if i == 0 else []))

    def conv_chunk(p, j):
        if True:
            wt, bw, wi = load_w()
            ps_c, Bc = ps_ring.next()
            mm_feat(ps_c, Bc, wt, bw, 0, xT_main, Bx_main, TG)
            ps_h, Bh = ps_ring.next()
            mm_feat(ps_h, Bh, wt, bw, 128, xT_main, Bx_main, TG)
            ps_b, Bb = ps_ring.next()
            mm_feat(ps_b, Bb, wt, bw, 256, xT_main, Bx_main, TG)
            u, Bu = ur.next()
            if p == 0:
                ps_x, Bxh = ps_ring.next()
                mm_feat(ps_x, Bxh, wt, bw, 0, lambda k: xT[:, k, HALO - 2:HALO], [Bx[0]], 2, pcol0=0)
                mm_feat(ps_x, Bxh, wt, bw, 128, lambda k: xT[:, k, HALO - 2:HALO], [Bx[0]], 2, pcol0=2)
                fw.op("act", lambda e: e.activation(out=chalo[:], in_=ps_x[:, 0:2], func=AF.Copy), r=[Bxh], w=[B_chalo])
                fw.op("dve", lambda e: e.tensor_tensor(out=u[:, 0:2], in0=chalo[:], in1=ps_x[:, 2:4], op=ALU.mult),
                      r=[B_chalo, Bxh], w=[Bu])
            else:
                fw.op("pool", lambda e, j=j: e.tensor_copy(out=u[:, 0:2], in_=carry[:, j, :]), r=[B_carry[j]], w=[Bu])
            cs, Bcs = csb.next()
            fw.op("act", lambda e: e.activation(out=cs[:], in_=ps_c[:], func=AF.Copy), r=[Bc], w=[Bcs])
            fw.op("dve", lambda e: e.tensor_tensor(out=u[:, 2:TG + 2], in0=cs[:], in1=ps_h[:], op=ALU.mult),
                  r=[Bcs, Bh], w=[Bu])
            fw.op("pool", lambda e, j=j: e.tensor_copy(out=carry[:, j, :], in_=u[:, TG:TG + 2]), r=[Bu], w=[B_carry[j]])
            y, By = yr.next()
            fw.op("act", lambda e, j=j: e.activation(out=y[:], in_=u[:, 0:TG], func=AF.Copy, scale=cwT[:, j, 0:1]),
                  r=[Bu, B_cwT], w=[By])
            fw.op("dve", lambda e, j=j: e.scalar_tensor_tensor(out=y[:], in0=u[:, 1:TG + 1], scalar=cwT[:, j, 1:2], in1=y[:],
                                                               op0=ALU.mult, op1=ALU.add), r=[Bu, B_cwT, By], w=[By])
            fw.op("dve", lambda e, j=j: e.scalar_tensor_tensor(out=y[:], in0=u[:, 2:TG + 2], scalar=cwT[:, j, 2:3], in1=y[:],
                                                               op0=ALU.mult, op1=ALU.add), r=[Bu, B_cwT, By], w=[By])
            fw.op("dve", lambda e, j=j: e.tensor_tensor(out=cvT[:, j, :], in0=ps_b[:], in1=y[:], op=ALU.mult),
                  r=[Bb, By], w=[B_cv[j]])
            w_release(wi)

    def steps1to4(p):
        for pc in range(2):
            wt, bw, wi = load_w()
            for cc in range(4):
                j = pc * 4 + cc
                ps, Bps = ps_ring.next()
                mm_feat(ps, Bps, wt, bw, cc * 128, xT_main, Bx_main, TG)
                fw.op("act", lambda e, j=j, ps=ps: e.activation(out=sgc[:, j, :], in_=ps[:], func=AF.Sigmoid), r=[Bps], w=[B_sgc[j]])
            w_release(wi)
        for pc in range(2):
            wt, bw, wi = load_w()
            for cc in range(4):
                i = pc * 4 + cc
                ps, Bps = ps_ring.next()
                mm_feat(ps, Bps, wt, bw, cc * 128, lambda k: cvT[:, k, :], B_cv, TG)
                fw.op("dve", lambda e, i=i, ps=ps: e.tensor_tensor(out=mrg[:, i, :], in0=ps[:], in1=sgc[:, i, :], op=ALU.mult),
                      r=[Bps, B_sgc[i]], w=[B_mrg[i]])
            w_release(wi)
        for pc in range(2):
            wt, bw, wi = load_w()
            for cc in range(4):
                c = pc * 4 + cc
                ps, Bps = ps_ring.next()
                mm_feat(ps, Bps, wt, bw, cc * 128, xT_main, Bx_main, TG)
                fw.op("act", lambda e, c=c, ps=ps: e.activation(out=qa[0:64, c, :], in_=ps[0:64, :], func=AF.Copy), r=[Bps], w=[B_qa[c]])
                fw.op("dve", lambda e, c=c, ps=ps: e.tensor_copy(out=qb[64:128, c, :], in_=ps[64:128, :]), r=[Bps], w=[B_qb[c]])
            w_release(wi)
        wt, bw, wi = load_w()
        for g in range(4):
            if p == 0:
                ps, Bps = ps_ring.next()
                mm_feat(ps, Bps, wt, bw, g * 128, lambda k: xT[:, k, 0:HALO], [Bx[0]], HALO)
                fw.op("dve", lambda e, g=g, ps=ps: e.tensor_copy(out=kT[:, g, 0:HALO], in_=ps[:, 0:HALO]), r=[Bps], w=[B_k[g]])
            else:
                fw.op("pool", lambda e, g=g: e.tensor_copy(out=kT[:, g, 0:HALO], in_=kT[:, g, TG:TG + HALO]), r=[B_k[g]], w=[B_k[g]])
            ps, Bps = ps_ring.next()
            mm_feat(ps, Bps, wt, bw, g * 128, xT_main, Bx_main, TG)
            fw.op("dve", lambda e, g=g, ps=ps: e.tensor_copy(out=kT[:, g, HALO:HALO + TG], in_=ps[:]), r=[Bps], w=[B_k[g]])
        w_release(wi)
        wt, bw, wi = load_w()
        for i in range(5):
            if i == 0 and p > 0:
                fw.op("pool", lambda e: e.tensor_copy(out=Vt[:, 0, :, :], in_=Vt[:, 4, :, :]), r=[B_V[4]], w=[B_V[0]])
                continue
            ps, Bps = ps_ring.next()
            for k in range(KC):
                fw.op("pe", lambda e, k=k, i=i, ps=ps, wt=wt: e.matmul(ps[:, 0:256], lhsT=xT[:, k, i * 128:(i + 1) * 128], rhs=wt[:, k, 0:256],
                                                                       start=(k == 0), stop=(k == KC - 1)),
                      r=[bw, Bx[i]], w=[Bps], inc=(k == KC - 1))
            fw.op("act", lambda e, i=i, ps=ps: e.activation(out=Vt[:, i, :, 0:64], in_=ps[:, 0:256].rearrange("p (g d) -> p g d", g=4),
                                                            func=AF.Copy), r=[Bps], w=[B_V[i]])
        w_release(wi)
        sga = cvT
        B_sga = B_cv
        for pc in range(2):
            wt, bw, wi = load_w()
            for cc in range(4):
                j = pc * 4 + cc
                ps, Bps = ps_ring.next()
                mm_feat(ps, Bps, wt, bw, cc * 128, xT_main, Bx_main, TG)
                fw.op("act", lambda e, j=j, ps=ps: e.activation(out=sga[:, j, :], in_=ps[:], func=AF.Sigmoid), r=[Bps], w=[B_sga[j]])
            w_release(wi)
        if p + 1 < NPASS:
            step0_load(p + 1)
        units = [(b, g, half) for b in range(4) for g in range(4) for half in range(2)]

        def scores(b, g, half):
            first = (p == 0 and b == 0)
            msk, Bm = (M2f, B_M2f) if first else (M2, B_M2)
            qc = slice(b * 128, (b + 1) * 128)
            qsrc = qa if half == 0 else qb
            Bqs = B_qa if half == 0 else B_qb
            ps, Bps = ps_ring.next()
            for kb in range(2):
                fw.op("pe", lambda e, kb=kb: e.matmul(ps[:, kb * 256:(kb + 1) * 256], lhsT=ident_b[:],
                                                      rhs=msk[:, 2 * kb:2 * kb + 2, :].rearrange("p a b -> p (a b)"),
                                                      start=True, stop=False), r=[B_identb, Bm], w=[Bps], inc=False)
                fw.op("pe", lambda e, kb=kb: e.matmul(
                    ps[:, kb * 256:(kb + 1) * 256], lhsT=kT[:, g, (b + kb) * 128:(b + kb + 1) * 128],
                    rhs=qsrc[:, 2 * g:2 * g + 2, qc], start=False, stop=True),
                      r=[B_k[g], Bqs[2 * g], Bqs[2 * g + 1]], w=[Bps], inc=(kb == 1))
            PT, BPT = PTr.next()
            fw.op("act", lambda e: e.activation(out=PT[:], in_=ps[:], func=AF.Exp, scale=0.125), r=[Bps], w=[BPT])
            return PT, BPT

        def pv(b, g, half, PT, BPT):
            for jj in range(2):
                s_ = 4 * g + 2 * half + jj
                oap, Bop = slot_ap(s_)
                for kb in range(2):
                    fw.op("pe", lambda e, kb=kb, jj=jj: e.matmul(
                        oap, lhsT=PT[:, kb * 256 + jj * 128: kb * 256 + (jj + 1) * 128], rhs=Vt[:, b + kb, g, :],
                        start=(kb == 0), stop=(kb == 1)),
                          r=[BPT, B_V[b + kb]], w=[Bop], inc=(kb == 1))

        def finish_block(b):
            ob, Bob = obr.next()
            for bk in range(3):
                n = 7 if bk < 2 else 2
                s0 = bk * 7
                pt, Bpt = ops[bk]
                v3 = pt[:, 0:n * 65].rearrange("p (s c) -> p s c", c=65)
                fw.op("dve", lambda e, v3=v3, s0=s0, n=n: e.tensor_tensor(out=den[:, s0:s0 + n].unsqueeze(2), in0=v3[:, :, 64:65],
                                                                           in1=esink[:, s0:s0 + n].unsqueeze(2), op=ALU.add),
                      r=[Bpt, B_esink], w=[B_den])
            fw.op("dve", lambda e: e.reciprocal(out=rden[:], in_=den[:]), r=[B_den], w=[B_rden])
            for bk in range(3):
                n = 7 if bk < 2 else 2
                s0 = bk * 7
                pt, Bpt = ops[bk]
                v3 = pt[:, 0:n * 65].rearrange("p (s c) -> p s c", c=65)
                fw.op("dve", lambda e, v3=v3, s0=s0, n=n: e.tensor_tensor(
                    out=ob[:, s0 * 64:(s0 + n) * 64].rearrange("p (s d) -> p s d", d=64), in0=v3[:, :, 0:64],
                    in1=rden[:, s0:s0 + n].unsqueeze(2).to_broadcast([128, n, 64]), op=ALU.mult),
                      r=[Bpt, B_rden], w=[Bob])
            return ob, Bob

        def transpose_block(b, ob, Bob):
            ps, Bps = ps_ring.next()
            psb = ps[:].bitcast(BF16).rearrange("p (c t) -> p c t", c=KC)
            for c in range(KC):
                fw.op("pe", lambda e, c=c: e.transpose(out=psb[:, c, :], in_=ob[:, c * 128:(c + 1) * 128], identity=ident_b[:]),
                      r=[Bob, B_identb], w=[Bps], inc=(c == KC - 1))
            fw.op("act", lambda e: e.activation(out=oT[:, :, b * 128:(b + 1) * 128], in_=psb, func=AF.Copy),
                  r=[Bps], w=[Bo[b]] + (Bx if b == 0 else []))

        pend = None
        pend_tr = None
        for ui, (b, g, half) in enumerate(units):
            PT, BPT = scores(b, g, half)
            if pend is not None:
                (pb_, pg_, ph_), pPT, pBPT = pend
                pv(pb_, pg_, ph_, pPT, pBPT)
                if pg_ == 3 and ph_ == 1:
                    if pend_tr is not None:
                        transpose_block(*pend_tr)
                    pend_tr = (pb_,) + finish_block(pb_)
            pend = ((b, g, half), PT, BPT)
        (pb_, pg_, ph_), pPT, pBPT = pend
        pv(pb_, pg_, ph_, pPT, pBPT)
        if pend_tr is not None:
            transpose_block(*pend_tr)
        transpose_block(pb_, *finish_block(pb_))
        for pc in range(2):
            wt, bw, wi = load_w()
            for cc in range(4):
                i = pc * 4 + cc
                ps, Bps = ps_ring.next()
                mm_feat(ps, Bps, wt, bw, cc * 128, lambda k: oT[:, k, 0:TG], Bo, TG)
                t1, Bt1 = t1r.next()
                fw.op("dve", lambda e, i=i, ps=ps, t1=t1: e.tensor_tensor(out=t1[:], in0=ps[:], in1=sga[:, i, :], op=ALU.mult),
                      r=[Bps, B_sga[i]], w=[Bt1])
                fw.op("pool", lambda e, i=i, t1=t1: e.tensor_tensor(out=mrg[:, i, :], in0=t1[:], in1=mrg[:, i, :], op=ALU.add),
                      r=[Bt1, B_mrg[i]], w=[B_mrg[i]])
            w_release(wi)

    def step5(p, wo, wg, nxt_conv=None):
        ctx = {}

        def load_x(t):
            tok0 = p * TG + t * 128
            xf, Bxf = xfr.next()
            fw.dma("sp", "xf_" + Bxf.name, xf[:], xin[HALO + tok0: HALO + tok0 + 128, :], w=[Bxf])
            pb_, Bpb = pbr.next()
            fw.dma("pool", "pb_" + Bpb.name, pb_[:], pin[tok0:tok0 + 128, :], w=[Bpb])
            ctx[("x", t)] = (xf, Bxf, pb_, Bpb)

        def stageA(t):
            tc_ = slice(t * 128, (t + 1) * 128)
            tok0 = p * TG + t * 128
            xf, Bxf, pb_, Bpb = ctx[("x", t)]
            z, Bz = zr.next()
            for h in range(2):
                ps, Bps = ps_ring.next()
                wt, bw, _ = wo[h]
                for k in range(KC):
                    fw.op("pe", lambda e, k=k, ps=ps, wt=wt: e.matmul(ps[:, :], lhsT=mrg[:, k, tc_], rhs=wt[:, k, :],
                                                                      start=(k == 0), stop=(k == KC - 1)),
                          r=[bw, B_mrg[k]], w=[Bps], inc=(k == KC - 1))
                fw.op("dve", lambda e, h=h, ps=ps: e.scalar_tensor_tensor(
                    out=z[:, h * 512:(h + 1) * 512], in0=xf[:, h * 512:(h + 1) * 512], scalar=ALPHA, in1=ps[:], op0=ALU.mult, op1=ALU.add),
                      r=[Bps, Bxf], w=[Bz])
                fw.op("dve", lambda e, h=h: e.bn_stats(out=stats[:, h, :], in_=z[:, h * 512:(h + 1) * 512]), r=[Bz], w=[B_stats])
            fw.op("dve", lambda e: e.bn_aggr(out=mv[:], in_=stats[:].rearrange("p a b -> p (a b)")), r=[B_stats], w=[B_mv])
            fw.op("act", lambda e: e.activation(out=st1[:, 0:1], in_=mv[:, 1:2], func=AF.Sqrt, bias=epsT[:, 0:1]), r=[B_mv, B_eps], w=[B_st1])
            fw.op("dve", lambda e: e.reciprocal(out=st1[:, 1:2], in_=st1[:, 0:1]), r=[B_st1], w=[B_st1])
            fw.op("dve", lambda e: e.tensor_scalar(out=st1[:, 2:3], in0=mv[:, 0:1], scalar1=st1[:, 1:2], scalar2=-1.0,
                                                   op0=ALU.mult, op1=ALU.mult), r=[B_st1, B_mv], w=[B_st1])
            fw.op("act", lambda e: e.activation(out=z[:], in_=z[:], func=AF.Identity, scale=st1[:, 1:2], bias=st1[:, 2:3]),
                  r=[Bz, B_st1], w=[Bz])
            fw.op("dve", lambda e: e.tensor_tensor(out=z[:], in0=z[:], in1=lng[:], op=ALU.mult), r=[Bz, B_lng], w=[Bz])
            fw.op("dve", lambda e: e.tensor_tensor(out=z[:], in0=z[:], in1=lnb[:], op=ALU.add), r=[Bz, B_lnb], w=[Bz])
            if debug:
                fw.dma("sp", "dbg", dbg_x1[tok0:tok0 + 128, :], z[:], r=[Bz], w=[B_dbg])
            x1b, Bx1b = x1br.next()
            fw.op("act", lambda e: e.activation(out=x1b[:], in_=z[:], func=AF.Copy), r=[Bz], w=[Bx1b])
            ctx[("A", t)] = (z, Bz, x1b, Bx1b, pb_, Bpb)
            if t + 2 < 4:
                load_x(t + 2)

        def stageBt(t):
            x1, Bz, x1b, Bx1b, pb_, Bpb = ctx[("A", t)]
            ps, Bps = ps_ring.next()
            psb = ps[:].bitcast(BF16).rearrange("p (c t) -> p c t", c=KC)
            for c in range(KC):
                fw.op("pe", lambda e, c=c: e.transpose(out=psb[:, c, :], in_=x1b[:, c * 128:(c + 1) * 128], identity=ident_b[:]),
                      r=[Bx1b, B_identb], w=[Bps], inc=(c == KC - 1))
            x1Tb, Bx1Tb = x1Tbr.next()
            fw.op("act", lambda e: e.activation(out=x1Tb[:], in_=psb, func=AF.Copy), r=[Bps], w=[Bx1Tb])
            for hh in range(2):
                ps, Bps = ps_ring.next()
                for c in range(4):
                    cc = hh * 4 + c
                    fw.op("pe", lambda e, c=c, cc=cc, ps=ps: e.transpose(out=ps[:, c * 128:(c + 1) * 128], in_=x1[:, cc * 128:(cc + 1) * 128],
                                                                         identity=ident_f[:]),
                          r=[Bz, B_identf], w=[Bps], inc=(c == 3))
                if hh == 0:
                    fw.op("act", lambda e, ps=ps: e.activation(out=x1Tf[:, 0:4, :], in_=ps[:].rearrange("p (c t) -> p c t", c=4), func=AF.Copy),
                          r=[Bps], w=[B_x1Tf])
                else:
                    fw.op("act", lambda e, ps=ps: e.activation(out=x1Tf[:, 4:8, :], in_=ps[:].rearrange("p (c t) -> p c t", c=4), func=AF.Copy),
                          r=[Bps], w=[B_x1Tf])
            ps, Bps = ps_ring.next()
            psb2 = ps[:].bitcast(BF16)
            for c in range(2):
                fw.op("pe", lambda e, c=c: e.transpose(out=psb2[:, c * 128:(c + 1) * 128], in_=pb_[:, c * 128:(c + 1) * 128], identity=ident_b[:]),
                      r=[Bpb, B_identb], w=[Bps], inc=(c == 1))
            pT, BpT = pTr.next()
            fw.op("act", lambda e: e.activation(out=pT[:], in_=psb2[:, 0:256].rearrange("p (c t) -> p c t", c=2), func=AF.Copy),
                  r=[Bps], w=[BpT])
            ps, Bps = ps_ring.next()
            for k in range(KC):
                fw.op("pe", lambda e, k=k: e.matmul(ps[:, 0:NE], lhsT=x1Tf[:, k, :], rhs=wr[:, k, :], start=(k == 0), stop=(k == KC - 1)),
                      r=[B_x1Tf, B_wr], w=[Bps], inc=(k == KC - 1))
            L, BL = Lr.next()
            fw.op("dve", lambda e: e.tensor_tensor(out=L[:], in0=ps[:, 0:NE], in1=brep[:], op=ALU.add), r=[Bps, B_brep], w=[BL])
            top8, Bt8 = top8r.next()
            fw.op("dve", lambda e: e.max(out=top8[:], in_=L[:]), r=[BL], w=[Bt8])
            mask, Bmk = maskr.next()
            fw.op("dve", lambda e: e.tensor_scalar(out=mask[:], in0=L[:], scalar1=top8[:, 3:4], scalar2=None, op0=ALU.is_ge),
                  r=[BL, Bt8], w=[Bmk])
            ctx[("Bm", t)] = (L, BL, top8, Bt8, mask, Bmk)
            ctx[("Bt", t)] = (x1Tb, Bx1Tb, pT, BpT)

        def stageBm(t):
            Tg = p * 4 + t
            tok0 = p * TG + t * 128
            x1, Bz, x1b, Bx1b, pb_, Bpb = ctx[("A", t)]
            x1Tb, Bx1Tb, pT, BpT = ctx[("Bt", t)]
            sg, Bsg = sgr.next()
            for h in range(2):
                ps, Bps = ps_ring.next()
                wt, bw, _ = wg[h]
                for k in range(KC):
                    fw.op("pe", lambda e, k=k, ps=ps, wt=wt: e.matmul(ps[:, :], lhsT=x1Tb[:, k, :], rhs=wt[:, k, :],
                                                                      start=(k == 0), stop=(k == KC - 1)),
                          r=[bw, Bx1Tb], w=[Bps], inc=(k == KC - 1))
                fw.op("act", lambda e, h=h, ps=ps: e.activation(out=sg[:, h * 512:(h + 1) * 512], in_=ps[:], func=AF.Sigmoid),
                      r=[Bps], w=[Bsg])
                ps2, Bps2 = ps_ring.next()
                for k in range(2):
                    fw.op("pe", lambda e, k=k, ps2=ps2, h=h: e.matmul(ps2[:, :], lhsT=pT[:, k, :], rhs=wpp[:, k, h * 512:(h + 1) * 512],
                                                                      start=(k == 0), stop=(k == 1)),
                          r=[B_wpp, BpT], w=[Bps2], inc=(k == 1))
                fw.op("dve", lambda e, h=h, ps2=ps2: e.tensor_tensor(out=sg[:, h * 512:(h + 1) * 512], in0=ps2[:], in1=sg[:, h * 512:(h + 1) * 512],
                                                                     op=ALU.mult), r=[Bps2, Bsg], w=[Bsg])
            fw.op("dve", lambda e: e.scalar_tensor_tensor(out=sg[:], in0=x1[:], scalar=ALPHA, in1=sg[:], op0=ALU.mult, op1=ALU.add),
                  r=[Bz, Bsg], w=[Bsg])
            if debug:
                fw.dma("sp", "dbg", dbg_acc[tok0:tok0 + 128, :], sg[:], r=[Bsg], w=[B_dbg])
            ctx[("sg", t)] = (sg, Bsg)

        def stageC(t):
            Tg = p * 4 + t
            x1, Bz, x1b, Bx1b, pb_, Bpb = ctx[("A", t)]
            L, BL, top8, Bt8, mask, Bmk = ctx[("Bm", t)]
            ps, Bps = ps_ring.next()
            fw.op("pe", lambda e: e.matmul(ps[:, 0:NE], lhsT=Umat[:], rhs=mask[:], start=True, stop=False),
                  r=[B_U, Bmk], w=[Bps], inc=False)
            fw.op("pe", lambda e: e.matmul(ps[:, 0:NE], lhsT=ones_f[:], rhs=cum[:], start=False, stop=True),
                  r=[B_ones, B_cum], w=[Bps])
            fw.op("pool", lambda e: e.tensor_tensor(out=cum[:], in0=cum[:], in1=mask[:], op=ALU.add), r=[B_cum, Bmk], w=[B_cum])
            fw.op("dve", lambda e: e.scalar_tensor_tensor(out=dest[:], in0=ps[:, 0:NE], scalar=float(CAP - 1), in1=ebase[:], op0=ALU.min, op1=ALU.add),
                  r=[Bps, B_ebase], w=[B_dest])
            fw.op("dve", lambda e: e.tensor_tensor(out=oh[:], in0=L[:].unsqueeze(1).to_broadcast([128, TOPK, NE]),
                                                   in1=top8[:, 0:TOPK].unsqueeze(2).to_broadcast([128, TOPK, NE]), op=ALU.is_equal),
                  r=[BL, Bt8], w=[B_oh])
            fw.op("dve", lambda e: e.tensor_tensor(out=prod[:], in0=oh[:], in1=dest[:].unsqueeze(1).to_broadcast([128, TOPK, NE]), op=ALU.mult),
                  r=[B_oh, B_dest], w=[B_prod])
            fw.op("dve", lambda e: e.tensor_reduce(out=dsel[:], in_=prod[:], axis=AX.X, op=ALU.add), r=[B_prod], w=[B_dsel])
            fw.op("dve", lambda e: e.tensor_copy(out=idx_all[:, Tg, :], in_=dsel[:]), r=[B_dsel], w=[B_idx])
            fw.op("dve", lambda e: e.tensor_scalar(out=st2[:, 3:4], in0=top8[:, 0:1], scalar1=-1.0, scalar2=None, op0=ALU.mult),
                  r=[Bt8], w=[B_st2])
            fw.op("act", lambda e: e.activation(out=e4[:], in_=top8[:, 0:TOPK], func=AF.Exp, bias=st2[:, 3:4], accum_out=st2[:, 4:5]),
                  r=[Bt8, B_st2], w=[B_e4, B_st2])
            fw.op("dve", lambda e: e.reciprocal(out=st2[:, 5:6], in_=st2[:, 4:5]), r=[B_st2], w=[B_st2])
            fw.op("dve", lambda e: e.tensor_scalar(out=w_all[:, Tg, :], in0=e4[:], scalar1=st2[:, 5:6], scalar2=None, op0=ALU.mult),
                  r=[B_e4, B_st2], w=[B_wall])
            fw.op("dve", lambda e: e.tensor_tensor(out=prod[:], in0=oh[:], in1=w_all[:, Tg, :].unsqueeze(2).to_broadcast([128, TOPK, NE]), op=ALU.mult),
                  r=[B_oh, B_wall], w=[B_prod])
            fw.op("dve", lambda e: e.tensor_reduce(out=Gt[:], in_=prod[:].rearrange("p k n -> p n k"), axis=AX.X, op=ALU.add),
                  r=[B_prod], w=[B_Gt])

            for k in range(TOPK):
                fw.dma("pool", "sc_" + Bx1b.name, r=[Bx1b, B_idx, B_xsz], w=[B_xs[Tg]],
                       fn=lambda e, k=k: e.indirect_dma_start(
                           out=xs_scr[:, :], out_offset=bass.IndirectOffsetOnAxis(ap=idx_all[:, Tg, k:k + 1], axis=0),
                           in_=x1b[:], in_offset=None))

        def stageC2(t):
            Tg = p * 4 + t
            sg, Bsg = ctx[("sg", t)]
            tok0 = p * TG + t * 128
            psg_, Bpsg_ = ps_ring.next()
            fw.op("pe", lambda e: e.transpose(out=psg_[0:NE, 0:128], in_=Gt[:], identity=ident_f[:]), r=[B_Gt, B_identf], w=[Bpsg_])
            GT, BGT = GTa.next()
            fw.op("act", lambda e: e.activation(out=GT[:], in_=psg_[0:NE, 0:128], func=AF.Copy), r=[Bpsg_], w=[BGT])
            for h in range(2):
                ps2, Bps2 = ps_ring.next()
                fw.op("pe", lambda e, h=h, ps2=ps2: e.matmul(ps2[:, :], lhsT=GT[:], rhs=bd[:, h * 512:(h + 1) * 512], start=True, stop=True),
                      r=[BGT, B_bd], w=[Bps2])
                fw.op("dve", lambda e, h=h, ps2=ps2: e.tensor_tensor(out=sg[:, h * 512:(h + 1) * 512], in0=ps2[:], in1=sg[:, h * 512:(h + 1) * 512], op=ALU.add),
                      r=[Bps2, Bsg], w=[Bsg])
            fw.dma("sp", "acc_" + Bsg.name, acc_scr[tok0:tok0 + 128, :], sg[:], r=[Bsg], w=[B_acc[Tg]])
        load_x(0)
        load_x(1)
        for t in range(4):
            stageA(t)
        w_release(wo[0][2], wo[1][2])
        nc_ = (lambda j: nxt_conv(j)) if nxt_conv is not None else (lambda j: None)
        nc_(0)
        stageBt(0)
        stageBt(1)
        nc_(1)
        stageBm(0)
        stageC(0)
        nc_(2)
        stageBt(2)
        stageBm(1)
        stageC2(0)
        stageC(1)
        stageBt(3)
        stageBm(2)
        stageC2(1)
        stageC(2)
        stageBm(3)
        stageC2(2)
        stageC(3)
        stageC2(3)
        w_release(wg[0][2], wg[1][2])

    fw.dma("pool", "wpp", wpp[:], w_pp.rearrange("(kc p) n -> p kc n", p=128), w=[B_wpp])
    step0_load(0)
    step0(0)
    def zero_init():
        z0, Bz0 = zr.items[0]
        fw.op("pool", lambda e: e.memset(z0[:], 0.0), w=[Bz0])
        z0b = z0[:].bitcast(BF16)
        for blk in range(XS_ROWS // 256):
            fw.dma("sp", "zi", xs_scr[blk * 256:(blk + 1) * 256, :].rearrange("(p r) d -> p (r d)", r=2), z0b,
                   r=[Bz0, B_cv[NEARLY - 1]], w=[B_xsz])
        fw.dma("sp", "zy", ys_scr[TRASH:TRASH + 128, :], z0[:], r=[Bz0], w=[B_ysz])

    NEARLY = 3
    for j in range(NEARLY):
        conv_chunk(0, j)
    zero_init()
    for p in range(NPASS):
        for j in range(NEARLY, KC):
            conv_chunk(p, j)
        steps1to4(p)
        wo = [load_w() for h in range(2)]
        wg = [load_w() for h in range(2)]
        if p + 1 < NPASS:
            step0(p + 1)
            step5(p, wo, wg, nxt_conv=lambda j, p=p: conv_chunk(p + 1, j))
        else:
            step5(p, wo, wg)

    if debug:
        fw.dma("sp", "dbg", dbg_idx[:, :, :], idx_all[:], r=[B_idx], w=[B_dbg])
        fw.dma("sp", "dbg", dbg_w[:, :, :], w_all[:], r=[B_wall], w=[B_dbg])

    fw.barrier()
    stack[0].close()
    stack[0] = ExitStack()
    ps_ring = Ring(banks)
    gur = Ring([T(f"wgu{i}", [128, KC, 2 * D], BF16) for i in range(2)])
    dnr = Ring([T(f"wdn{i}", [128, KC, D], BF16) for i in range(2)])
    xstr = Ring([T(f"xst{i}", [128, NSL, D], BF16) for i in range(2)])
    xsTr = Ring([T(f"xsT{i}", [128, KC, CAP], BF16) for i in range(2)])
    hTr = Ring([T(f"hT{i}", [128, KC, CAP], BF16) for i in range(2)])
    gr = Ring([T(f"gg{i}", [128, CAP], F32) for i in range(2)])
    sr = Ring([T(f"ss{i}", [128, CAP], F32) for i in range(2)])
    tr = Ring([T(f"tt{i}", [128, CAP], F32) for i in range(2)])
    ysr = Ring([T(f"ysb{i}", [128, NSL, D], F32) for i in range(2)])
    B_ys = [Buf(f"ys{e}") for e in range(NE)]
    NP_ = SLT[0]

    def load_expert(e_):
        wgt, Bwg = gur.next()
        wdt, Bwd = dnr.next()
        fw.dma("pool", "g_" + Bwg.name, wgt[:], w_gu[e_].rearrange("(kc p) n -> p kc n", p=128), w=[Bwg])
        fw.dma("pool", "d_" + Bwd.name, wdt[:], w_down[e_].rearrange("(kc p) n -> p kc n", p=128), w=[Bwd])
        return (wgt, Bwg, wdt, Bwd)

    def load_xs(e_):
        xst, Bxst = xstr.next()
        fw.dma("sp", "xs_" + Bxst.name, xst[0:NP_, :, :], xs_scr[e_ * CAP:(e_ + 1) * CAP, :].rearrange("(p s) d -> p s d", s=NSL),
               r=B_xs, w=[Bxst])
        return xst, Bxst

    def do_transposes(xst, Bxst):
        xsT, BxsT = xsTr.next()
        for s in range(NSL):
            ps, Bps = ps_ring.next()
            psb = ps[:].bitcast(BF16).rearrange("p (c t) -> p c t", c=KC)
            n_ = SLT[s]
            for c in range(KC):
                fw.op("pe", lambda e, c=c, s=s, psb=psb, n_=n_: e.transpose(out=psb[:, c, 0:n_], in_=xst[0:n_, s, c * 128:(c + 1) * 128], identity=ident_b[0:n_, 0:n_]),
                      r=[Bxst, B_identb], w=[Bps], inc=(c == KC - 1))
            if s % 2 == 0:
                fw.op("act", lambda e, s=s, psb=psb, n_=n_: e.activation(out=xsT[:, :, s * n_:(s + 1) * n_], in_=psb[:, :, 0:n_], func=AF.Copy), r=[Bps], w=[BxsT])
            else:
                fw.op("dve", lambda e, s=s, psb=psb, n_=n_: e.tensor_copy(out=xsT[:, :, s * n_:(s + 1) * n_], in_=psb[:, :, 0:n_]), r=[Bps], w=[BxsT])
        return xsT, BxsT

    wts = {0: load_expert(0)}
    xss = {0: load_xs(0), 1: load_xs(1)}
    cur_T = do_transposes(*xss[0])
    for e_ in range(NE):
        wgt_, Bwg_, wd_, Bwd_ = wts.pop(e_)
        Bwu_ = Bwg_
        wg_ = wgt_[:, :, 0:D]
        wu_ = wgt_[:, :, D:2 * D]
        if e_ + 1 < NE:
            wts[e_ + 1] = load_expert(e_ + 1)
        xsT, BxsT = cur_T
        hT, BhT = hTr.next()
        for j in range(KC):
            psg, Bpg = ps_ring.next()
            for k in range(KC):
                fw.op("pe", lambda e, k=k, j=j, psg=psg: e.matmul(psg[:, 0:CAP], lhsT=wg_[:, k, j * 128:(j + 1) * 128], rhs=xsT[:, k, :],
                                                                 start=(k == 0), stop=(k == KC - 1)),
                      r=[Bwg_, BxsT], w=[Bpg], inc=(k == KC - 1))
            psu, Bpu = ps_ring.next()
            for k in range(KC):
                fw.op("pe", lambda e, k=k, j=j, psu=psu: e.matmul(psu[:, 0:CAP], lhsT=wu_[:, k, j * 128:(j + 1) * 128], rhs=xsT[:, k, :],
                                                                 start=(k == 0), stop=(k == KC - 1)),
                      r=[Bwu_, BxsT], w=[Bpu], inc=(k == KC - 1))
            gg, Bgg = gr.next()
            ss, Bss = sr.next()
            tt, Btt = tr.next()
            fw.op("dve", lambda e, j=j, psg=psg, gg=gg: e.tensor_scalar(out=gg[:], in0=psg[:, 0:CAP], scalar1=bguT[:, j, e_:e_ + 1], scalar2=SW_LIMIT,
                                                                     op0=ALU.add, op1=ALU.min), r=[Bpg, B_bguT], w=[Bgg])
            fw.op("act", lambda e, gg=gg, ss=ss: e.activation(out=ss[:], in_=gg[:], func=AF.Sigmoid, scale=SW_ALPHA), r=[Bgg], w=[Bss])
            fw.op("dve", lambda e, j=j, psu=psu, tt=tt: e.tensor_scalar(out=tt[:], in0=psu[:, 0:CAP], scalar1=bu1T[:, j, e_:e_ + 1], scalar2=SW_LIMIT + 1.0,
                                                                     op0=ALU.add, op1=ALU.min), r=[Bpu, B_bu1T], w=[Btt])
            fw.op("dve", lambda e, gg=gg, ss=ss: e.tensor_tensor(out=ss[:], in0=gg[:], in1=ss[:], op=ALU.mult), r=[Bgg, Bss], w=[Bss])
            fw.op("dve", lambda e, j=j, tt=tt, ss=ss: e.scalar_tensor_tensor(out=hT[:, j, :], in0=tt[:], scalar=1.0 - SW_LIMIT, in1=ss[:],
                                                                           op0=ALU.max, op1=ALU.mult), r=[Btt, Bss], w=[BhT])
        if e_ + 1 < NE:
            cur_T = do_transposes(*xss.pop(e_ + 1))
            if e_ + 2 < NE:
                xss[e_ + 2] = load_xs(e_ + 2)
        ysb, Bysb = ysr.next()
        for s in range(NSL):
            n_ = SLT[s]
            for h in range(2):
                ps, Bps = ps_ring.next()
                for k in range(KC):
                    fw.op("pe", lambda e, k=k, s=s, h=h, ps=ps, n_=n_: e.matmul(ps[0:n_, :], lhsT=hT[:, k, s * n_:(s + 1) * n_], rhs=wd_[:, k, h * 512:(h + 1) * 512],
                                                                               start=(k == 0), stop=(k == KC - 1)),
                          r=[Bwd_, BhT], w=[Bps], inc=(k == KC - 1))
                fw.op("act", lambda e, s=s, h=h, ps=ps, n_=n_: e.activation(out=ysb[0:n_, s, h * 512:(h + 1) * 512], in_=ps[0:n_, :], func=AF.Copy), r=[Bps], w=[Bysb])
        fw.dma("act", "ys_" + Bysb.name, ys_scr[e_ * CAP:(e_ + 1) * CAP, :].rearrange("(p s) d -> p s d", s=NSL), ysb[0:NP_, :, :],
               r=[Bysb], w=[B_ys[e_]])

    fw.barrier()
    stack[0].close()
    stack[0] = ExitStack()
    accr = Ring([T(f"acc{i}", [128, D], F32) for i in range(6)])
    gkr = Ring([T(f"gk{i}", [128, D], F32) for i in range(24)])
    fw.dma("sp", "c2", lng[:], ln2_g.partition_broadcast(128), w=[B_lng])
    fw.dma("sp", "c2", lnb[:], ln2_b.partition_broadcast(128), w=[B_lnb])
    for b_ in (B_lng, B_lnb):
        b_.lw = ("c2", fw.cnt["c2"])
    B_out = [Buf(f"out{t}") for t in range(16)]
    st3r = Ring([T(f"st3_{i}", [128, 8], F32) for i in range(3)])
    mv3r = Ring([T(f"mv3_{i}", [128, 2], F32) for i in range(3)])
    stats3r = Ring([T(f"stats3_{i}", [128, 2, 6], F32) for i in range(3)])
    pre = {}

    def prefetch_c(Tg):
        tok0 = Tg * 128
        acc, Bacc = accr.next()
        fw.dma("sp", "la_" + Bacc.name, acc[:], acc_scr[tok0:tok0 + 128, :], r=[B_acc[Tg]], w=[Bacc])
        gks = []
        for k in range(TOPK):
            gk, Bgk = gkr.next()
            fw.dma("pool", "ga_" + Bgk.name, r=B_ys + [B_ysz, B_idx], w=[Bgk],
                   fn=lambda e, k=k, gk=gk: e.indirect_dma_start(
                       out=gk[:], out_offset=None, in_=ys_scr[:, :],
                       in_offset=bass.IndirectOffsetOnAxis(ap=idx_all[:, Tg, k:k + 1], axis=0)))
            gks.append((gk, Bgk))
        pre[Tg] = (acc, Bacc, gks)

    PFD = 4
    for Tg in range(PFD):
        prefetch_c(Tg)

    def q1(Tg):
        if Tg + PFD < 16:
            prefetch_c(Tg + PFD)
        acc, Bacc, gks = pre[Tg]
        for k in range(TOPK):
            gk, Bgk = gks[k]
            fw.op("dve", lambda e, k=k, gk=gk: e.scalar_tensor_tensor(out=acc[:], in0=gk[:], scalar=w_all[:, Tg, k:k + 1], in1=acc[:],
                                                                     op0=ALU.mult, op1=ALU.add), r=[Bgk, B_wall, Bacc], w=[Bacc])
        st, Bst = st3r.next()
        mvx, Bmvx = mv3r.next()
        sts, Bsts = stats3r.next()
        for h in range(2):
            fw.op("dve", lambda e, h=h: e.bn_stats(out=sts[:, h, :], in_=acc[:, h * 512:(h + 1) * 512]), r=[Bacc], w=[Bsts])
        fw.op("dve", lambda e: e.bn_aggr(out=mvx[:], in_=sts[:].rearrange("p a b -> p (a b)")), r=[Bsts], w=[Bmvx])
        fw.op("act", lambda e: e.activation(out=st[:, 0:1], in_=mvx[:, 1:2], func=AF.Sqrt, bias=epsT[:, 0:1]), r=[Bmvx, B_eps], w=[Bst])
        fw.op("dve", lambda e: e.reciprocal(out=st[:, 1:2], in_=st[:, 0:1]), r=[Bst], w=[Bst])
        fw.op("dve", lambda e: e.tensor_scalar(out=st[:, 2:3], in0=mvx[:, 0:1], scalar1=st[:, 1:2], scalar2=-1.0, op0=ALU.mult, op1=ALU.mult),
              r=[Bst, Bmvx], w=[Bst])
        fw.op("act", lambda e: e.activation(out=acc[:], in_=acc[:], func=AF.Identity, scale=st[:, 1:2], bias=st[:, 2:3]),
              r=[Bacc, Bst], w=[Bacc])
        fw.op("pool", lambda e: e.tensor_tensor(out=acc[:], in0=acc[:], in1=lng[:], op=ALU.mult), r=[Bacc, B_lng], w=[Bacc])

    def q2(Tg):
        tok0 = Tg * 128
        acc, Bacc, gks = pre.pop(Tg)
        fw.op("dve", lambda e: e.tensor_tensor(out=acc[:], in0=acc[:], in1=lnb[:], op=ALU.add), r=[Bacc, B_lnb], w=[Bacc])
        fw.dma("act", "out_" + Bacc.name, out[tok0:tok0 + 128, :], acc[:], r=[Bacc], w=[B_out[Tg]])

    q1(0)
    for Tg in range(16):
        if Tg + 1 < 16:
            q1(Tg + 1)
        q2(Tg)
    fw.barrier()
    stack[0].close()
    persist_stack.close()
    return nc


def _prep_inputs(inp):
    f = lambda a: np.ascontiguousarray(np.asarray(a, dtype=np.float32))
    x = f(inp["x"]); p = f(inp["p"])[0]
    w_in = f(inp["w_in"])[0]
    Bc, Cc, Hc = w_in[:, 0:1024], w_in[:, 1024:2048], w_in[:, 2048:3072]
    Q, K, V = w_in[:, 3072:4096], w_in[:, 4096:4352], w_in[:, 4352:4608]
    GC, GA = w_in[:, 4608:5632], w_in[:, 5632:6656]
    cols = []
    for j in range(8):
        sl = slice(j * 128, (j + 1) * 128)
        cols += [Cc[:, sl], Hc[:, sl], Bc[:, sl]]
    cols.append(GC); cols.append(Q)
    for g in range(4):
        cols += [K[:, g * 64:(g + 1) * 64], K[:, g * 64:(g + 1) * 64]]
    cols.append(V); cols.append(GA)
    w_in_p = np.ascontiguousarray(np.concatenate(cols, axis=1))
    assert w_in_p.shape == (D, WIN_COLS)
    rows = np.concatenate([np.arange(h * 64, (h + 1) * 64) for h in HEAD_PERM])
    shared = {
        "w_in": w_in_p,
        "conv_w": f(inp["conv_w"])[0],
        "w_brc": f(inp["w_br_conv"])[0],
        "w_bra": np.ascontiguousarray(f(inp["w_br_attn"])[0][rows]),
        "sinks": np.ascontiguousarray(f(inp["attn_sinks"])[0][HEAD_PERM]),
        "w_out": f(inp["w_out"])[0],
        "ln1_g": f(inp["ln1_g"])[0], "ln1_b": f(inp["ln1_b"])[0],
        "w_router": f(inp["w_router"])[0], "b_router": f(inp["b_router"])[0],
        "w_gu": f(inp["w_gu"])[0], "b_gu": f(inp["b_gu"])[0],
        "w_down": f(inp["w_down"])[0], "b_down": f(inp["b_down"])[0],
        "w_pp": f(inp["w_ple_proj"])[0], "w_pg": f(inp["w_ple_gate"])[0],
        "ln2_g": f(inp["ln2_g"])[0], "ln2_b": f(inp["ln2_b"])[0],
    }
    in_maps = []
    for c in range(NCORES):
        b = c // 4
        s0 = (c % 4) * NT
        halo = x[b, s0 - HALO:s0] if s0 > 0 else np.zeros((HALO, D), np.float32)
        m = dict(shared)
        m["xin"] = np.ascontiguousarray(np.concatenate([halo, x[b, s0:s0 + NT]], axis=0))
        m["pin"] = np.ascontiguousarray(p[b, s0:s0 + NT])
        m["negflag"] = np.full((128, 1), NEG if s0 == 0 else 0.0, np.float32)
        in_maps.append(m)
    return in_maps


def kernel(**inputs):
    in_maps = _prep_inputs(inputs)
    nc = build_program(debug=False)
    res = run_bass_kernel_spmd(nc, in_maps, core_ids=list(range(NCORES)))
    outs = [np.asarray(r["out"], dtype=np.float32) for r in res.results]
    full = np.stack([np.concatenate(outs[0:4], axis=0), np.concatenate(outs[4:8], axis=0)], axis=0)
    return full.astype(np.float32)
```
